# Optimizing a Trainium2 kernel written in Bass

```python
import jax, jax.numpy as jnp
from jax import lax
import numpy as np

D_MODEL = 2048
BATCH = 8
SEQ = 2048
DEPTH = 1

GRID_W = 64
CTX_LEN = 256
D_MIX = D_MODEL
D_MLSTM = D_MIX // 2
D_CMLP = D_MIX - D_MLSTM
MLSTM_HEADS = 4
MLSTM_HD = D_MLSTM // MLSTM_HEADS
MLSTM_CHUNK = 128
CONV_K = 3
CMLP_GROUPS = 4
CMLP_GD = D_CMLP // CMLP_GROUPS
CMLP_CHUNK = 128
N_GROUPS = 4
EXPERTS_PER_GROUP = 8
N_EXPERTS = N_GROUPS * EXPERTS_PER_GROUP
TOP_K_INNER = 2
D_EXPERT = D_MODEL // 2
MOE_BLOCK = 128
N_MOD = 6
N_GATE_COLS = 4 * MLSTM_HEADS
D_IN = 4 * D_MLSTM + N_GATE_COLS + 2 * D_CMLP
DEEPNORM_ALPHA = (2.0 * DEPTH) ** 0.25
DEEPNORM_BETA = (8.0 * DEPTH) ** -0.25
LN_EPS = 1e-6

kernel_name = 'hybrid_mlstm_chunkmlp_hmoe_dit_block'

F32 = jnp.float32


def _layer_norm(x, w, b):
    xf = x.astype(F32)
    mu = xf.mean(-1, keepdims=True)
    var = jnp.mean(jnp.square(xf - mu), -1, keepdims=True)
    y = (xf - mu) * lax.rsqrt(var + LN_EPS)
    return (y * w.astype(F32) + b.astype(F32)).astype(x.dtype)


def _grid_pos_embed(rows):
    r = jnp.repeat(jnp.arange(rows, dtype=F32), GRID_W)
    col = jnp.tile(jnp.arange(GRID_W, dtype=F32), rows)
    quarter = D_MODEL // 4
    omega = 1.0 / (10000.0 ** (jnp.arange(quarter, dtype=F32) / quarter))
    ar = r[:, None] * omega
    ac = col[:, None] * omega
    return jnp.concatenate([jnp.sin(ar), jnp.cos(ar), jnp.sin(ac), jnp.cos(ac)], -1)


def _dwconv_centred(x, w, b):
    ch = x.shape[-1]
    y = lax.conv_general_dilated(x, w[:, None, :].astype(x.dtype), window_strides=(1,),
                                 padding=[(CONV_K // 2, CONV_K // 2)],
                                 dimension_numbers=('NWC', 'WIO', 'NWC'), feature_group_count=ch)
    return y + b.astype(x.dtype)


def _heads(t):
    bsz, n, _ = t.shape
    return t.reshape(bsz, n, MLSTM_HEADS, MLSTM_HD).transpose(0, 2, 1, 3)


def _stream_proj(h, w_in, conv_w, conv_b, gate_bias):
    bsz, n, _ = h.shape
    p = jnp.einsum('bld,de->ble', h, w_in)
    dm = D_MLSTM
    qk = jax.nn.silu(_dwconv_centred(p[..., :2 * dm], conv_w, conv_b))
    q = _heads(qk[..., :dm])
    k = _heads(qk[..., dm:]) * (MLSTM_HD ** -0.5)
    v = _heads(p[..., 2 * dm:3 * dm])
    o = p[..., 3 * dm:4 * dm]
    g = p[..., 4 * dm:4 * dm + N_GATE_COLS].astype(F32).reshape(bsz, n, 4, MLSTM_HEADS)
    g = (g + gate_bias.astype(F32)).transpose(2, 0, 3, 1)
    gates = (g[0], jax.nn.log_sigmoid(g[1]), g[2], jax.nn.log_sigmoid(g[3]))
    uv = jax.nn.gelu(p[..., 4 * dm + N_GATE_COLS:])
    return q, k, v, o, gates, uv[..., :D_CMLP], uv[..., D_CMLP:]


def _zero_state(bsz):
    return (jnp.zeros((bsz, MLSTM_HEADS, MLSTM_HD, MLSTM_HD), F32),
            jnp.zeros((bsz, MLSTM_HEADS, MLSTM_HD), F32),
            jnp.zeros((bsz, MLSTM_HEADS), F32))


def _chunk_states(k, v, log_i, log_f, state):
    bsz, nh, n, dh = k.shape
    nc = n // MLSTM_CHUNK
    kc = k.astype(F32).reshape(bsz, nh, nc, MLSTM_CHUNK, dh)
    vc = v.astype(F32).reshape(bsz, nh, nc, MLSTM_CHUNK, dh)
    li = log_i.reshape(bsz, nh, nc, MLSTM_CHUNK)
    b = lax.cumsum(log_f.reshape(bsz, nh, nc, MLSTM_CHUNK), axis=3)
    b_last = b[..., -1]
    a = b_last[..., None] - b + li
    m_loc = a.max(-1)
    w = jnp.exp(a - m_loc[..., None])
    c_loc = jnp.einsum('bhcsd,bhcse->bhcde', vc * w[..., None], kc)
    n_loc = jnp.einsum('bhcs,bhcse->bhce', w, kc)

    def step(carry, inp):
        cm, nv, m = carry
        cl, nl, ml, bl = inp
        m_new = jnp.maximum(bl + m, ml)
        s_old = jnp.exp(bl + m - m_new)
        s_loc = jnp.exp(ml - m_new)
        c_new = s_old[..., None, None] * cm + s_loc[..., None, None] * cl
        n_new = s_old[..., None] * nv + s_loc[..., None] * nl
        return (c_new, n_new, m_new), (cm, nv, m)

    xs = (jnp.moveaxis(c_loc, 2, 0), jnp.moveaxis(n_loc, 2, 0),
          jnp.moveaxis(m_loc, 2, 0), jnp.moveaxis(b_last, 2, 0))
    final, (c_in, n_in, m_in) = lax.scan(step, state, xs)
    c_in = jnp.moveaxis(c_in, 0, 2)
    n_in = jnp.moveaxis(n_in, 0, 2)
    m_in = jnp.moveaxis(m_in, 0, 2)
    return kc, vc, li, b, c_in, n_in, m_in, final


def _mlstm_core(q, k, v, log_i, log_f, state):
    kc, vc, li, b, c_in, n_in, m_in, final = _chunk_states(k, v, log_i, log_f, state)
    bsz, nh, nc, lc, dh = kc.shape
    qc = q.astype(F32).reshape(bsz, nh, nc, lc, dh)
    order = jnp.tril(jnp.ones((lc, lc), bool))
    dmat = jnp.where(order, b[..., :, None] - b[..., None, :] + li[..., None, :], -jnp.inf)
    inter = b + m_in[..., None]
    m_t = jnp.maximum(inter, dmat.max(-1))
    s_inter = jnp.exp(inter - m_t)
    pmat = jnp.exp(dmat - m_t[..., None]) * jnp.einsum('bhctd,bhcsd->bhcts', qc, kc)
    num = (jnp.einsum('bhcts,bhcsd->bhctd', pmat, vc)
           + s_inter[..., None] * jnp.einsum('bhcde,bhcte->bhctd', c_in, qc))
    den = pmat.sum(-1) + s_inter * jnp.einsum('bhce,bhcte->bhct', n_in, qc)
    h = num / jnp.maximum(jnp.abs(den), jnp.exp(-m_t))[..., None]
    return h.reshape(bsz, nh, nc * lc, dh), final


def _flip(t):
    return jnp.flip(t, axis=2)


def _mlstm_bidir(q, k, v, gates, init_f, init_b):
    li_f, lf_f, li_b, lf_b = gates
    h_f, fin_f = _mlstm_core(q, k, v, li_f, lf_f, init_f)
    h_b, fin_b = _mlstm_core(_flip(q), _flip(k), _flip(v), _flip(li_b), _flip(lf_b), init_b)
    return h_f + _flip(h_b), fin_f, fin_b


def _mlstm_bidir_final(k, v, gates, init):
    li_f, lf_f, li_b, lf_b = gates
    fin_f = _chunk_states(k, v, li_f, lf_f, init)[-1]
    fin_b = _chunk_states(_flip(k), _flip(v), _flip(li_b), _flip(lf_b), init)[-1]
    return fin_f, fin_b


def _chunk_mlp(u, vg, norm_w, w_s, b_s):
    bsz, n, _ = u.shape
    shp = (bsz, n // CMLP_CHUNK, CMLP_CHUNK, CMLP_GROUPS, CMLP_GD)
    vf = vg.astype(F32).reshape(shp)
    mu = vf.mean(-1, keepdims=True)
    var = jnp.mean(jnp.square(vf - mu), -1, keepdims=True)
    vn = ((vf - mu) * lax.rsqrt(var + LN_EPS) * norm_w.astype(F32).reshape(CMLP_GROUPS, CMLP_GD)).astype(u.dtype)
    s = jnp.einsum('gpq,bcqgd->bcpgd', w_s, vn) + b_s.T[:, :, None]
    return (u.reshape(shp) * s).reshape(bsz, n, D_CMLP)


def _mixer_out(h_m, o, u, vg, mlstm_norm_w, cmlp_norm_w, w_s, b_s, w_out):
    bsz, nh, n, dh = h_m.shape
    mu = h_m.mean(-1, keepdims=True)
    var = jnp.mean(jnp.square(h_m - mu), -1, keepdims=True)
    hn = (h_m - mu) * lax.rsqrt(var + LN_EPS) * mlstm_norm_w.astype(F32).reshape(nh, 1, dh)
    hn = hn.transpose(0, 2, 1, 3).reshape(bsz, n, D_MLSTM).astype(o.dtype)
    y_m = hn * jax.nn.sigmoid(o)
    y_c = _chunk_mlp(u, vg, cmlp_norm_w, w_s, b_s)
    return jnp.einsum('ble,ed->bld', jnp.concatenate([y_m, y_c], -1), w_out)


def _hier_moe(h, r1_w, r1_b, r2_w, r2_b, w_gate, w_up, w_down):
    lead = h.shape[:-1]
    t = h.reshape(-1, D_MODEL)
    n_tok = t.shape[0]
    logits1 = (t @ r1_w + r1_b).astype(F32)
    grp = jnp.argmax(logits1, -1)
    p_grp = jnp.take_along_axis(jax.nn.softmax(logits1, -1), grp[:, None], -1)[:, 0]
    logits2 = (t @ r2_w + r2_b).astype(F32).reshape(n_tok, N_GROUPS, EXPERTS_PER_GROUP)
    l2 = jnp.take_along_axis(logits2, grp[:, None, None], 1)[:, 0]
    top_v, top_i = lax.top_k(l2, TOP_K_INNER)
    gate = p_grp[:, None] * jax.nn.softmax(top_v, -1)
    expert = grp[:, None] * EXPERTS_PER_GROUP + top_i
    n_asg = n_tok * TOP_K_INNER
    e_flat = expert.reshape(-1).astype(jnp.int32)
    tok = jnp.repeat(jnp.arange(n_tok, dtype=jnp.int32), TOP_K_INNER)
    order = jnp.argsort(e_flat)
    e_s, tok_s, g_s = e_flat[order], tok[order], gate.reshape(-1)[order]
    counts = jnp.zeros((N_EXPERTS,), jnp.int32).at[e_flat].add(1)
    start = jnp.cumsum(counts) - counts
    padded = (counts + MOE_BLOCK - 1) // MOE_BLOCK * MOE_BLOCK
    pad_end = jnp.cumsum(padded)
    pad_start = pad_end - padded
    pos = pad_start[e_s] + (jnp.arange(n_asg, dtype=jnp.int32) - start[e_s])
    n_blk = (n_asg + MOE_BLOCK - 1) // MOE_BLOCK + N_EXPERTS
    xp = jnp.zeros((n_blk * MOE_BLOCK, D_MODEL), t.dtype).at[pos].set(t[tok_s])
    blk_e = jnp.minimum(jnp.searchsorted(pad_end, jnp.arange(n_blk, dtype=jnp.int32) * MOE_BLOCK, side='right'),
                        N_EXPERTS - 1)

    def expert_block(args):
        xb, e = args
        return (jax.nn.silu(xb @ w_gate[e]) * (xb @ w_up[e])) @ w_down[e]

    yp = lax.map(expert_block, (xp.reshape(n_blk, MOE_BLOCK, D_MODEL), blk_e))
    ys = yp.reshape(n_blk * MOE_BLOCK, D_MODEL)[pos] * g_s[:, None].astype(t.dtype)
    out = jax.ops.segment_sum(ys, tok_s, num_segments=n_tok)
    return out.reshape(*lead, D_MODEL)


def setup_inputs(seed: int = 0) -> dict:
    key = jax.random.key(seed)
    ks = jax.random.split(key, 32)
    dm = D_MODEL

    def nrm(k, shape, s):
        return jax.random.normal(k, shape, F32) * s

    f_bias = jnp.linspace(3.0, 6.0, MLSTM_HEADS, dtype=F32)
    i_bias = jnp.zeros((MLSTM_HEADS,), F32)
    gate_base = jnp.stack([i_bias, f_bias, i_bias, f_bias])
    return {
        'x': nrm(ks[0], (BATCH, SEQ, dm), 1.0),
        'c': nrm(ks[1], (BATCH, dm), 1.0),
        'ctx': nrm(ks[2], (BATCH, CTX_LEN, dm), 1.0),
        'c_ctx': nrm(ks[3], (dm,), 1.0),
        'w_mod': nrm(ks[4], (DEPTH, dm, N_MOD * dm), 0.5 * dm ** -0.5),
        'b_mod': nrm(ks[5], (DEPTH, N_MOD * dm), 0.01),
        'w_in': nrm(ks[6], (DEPTH, dm, D_IN), dm ** -0.5),
        'conv_w': nrm(ks[7], (DEPTH, CONV_K, 2 * D_MLSTM), CONV_K ** -0.5),
        'conv_b': nrm(ks[8], (DEPTH, 2 * D_MLSTM), 0.01),
        'gate_bias': gate_base[None] + nrm(ks[9], (DEPTH, 4, MLSTM_HEADS), 0.1),
        'mlstm_norm_w': 1.0 + nrm(ks[10], (DEPTH, D_MLSTM), 0.02),
        'cmlp_norm_w': 1.0 + nrm(ks[11], (DEPTH, D_CMLP), 0.02),
        'w_s': nrm(ks[12], (DEPTH, CMLP_GROUPS, CMLP_CHUNK, CMLP_CHUNK), CMLP_CHUNK ** -0.5),
        'b_s': 1.0 + nrm(ks[13], (DEPTH, CMLP_GROUPS, CMLP_CHUNK), 0.02),
        'w_out': nrm(ks[14], (DEPTH, D_MIX, dm), DEEPNORM_BETA * D_MIX ** -0.5),
        'ln1_w': 1.0 + nrm(ks[15], (DEPTH, dm), 0.02),
        'ln1_b': nrm(ks[16], (DEPTH, dm), 0.02),
        'router1_w': nrm(ks[17], (DEPTH, dm, N_GROUPS), dm ** -0.5),
        'router1_b': nrm(ks[18], (DEPTH, N_GROUPS), 0.01),
        'router2_w': nrm(ks[19], (DEPTH, dm, N_EXPERTS), dm ** -0.5),
        'router2_b': nrm(ks[20], (DEPTH, N_EXPERTS), 0.01),
        'w_gate': nrm(ks[21], (DEPTH, N_EXPERTS, dm, D_EXPERT), dm ** -0.5),
        'w_up': nrm(ks[22], (DEPTH, N_EXPERTS, dm, D_EXPERT), dm ** -0.5),
        'w_down': nrm(ks[23], (DEPTH, N_EXPERTS, D_EXPERT, dm), DEEPNORM_BETA * D_EXPERT ** -0.5),
        'ln2_w': 1.0 + nrm(ks[24], (DEPTH, dm), 0.02),
        'ln2_b': nrm(ks[25], (DEPTH, dm), 0.02),
    }


def reference(x, c, ctx, c_ctx, w_mod, b_mod, w_in, conv_w, conv_b, gate_bias,
              mlstm_norm_w, cmlp_norm_w, w_s, b_s, w_out, ln1_w, ln1_b,
              router1_w, router1_b, router2_w, router2_b, w_gate, w_up, w_down,
              ln2_w, ln2_b):
    bsz, n_lat, dm = x.shape
    rows = n_lat // GRID_W
    x = x + _grid_pos_embed(rows).astype(x.dtype)[None]
    for layer in range(DEPTH):
        last = layer == DEPTH - 1
        mod_x = (jax.nn.silu(c) @ w_mod[layer] + b_mod[layer]).reshape(bsz, N_MOD, 1, dm)
        mod_c = (jax.nn.silu(c_ctx) @ w_mod[layer] + b_mod[layer]).reshape(N_MOD, 1, dm)
        hx = x * (1 + mod_x[:, 1]) + mod_x[:, 0]
        hc = ctx * (1 + mod_c[1]) + mod_c[0]
        qx, kx, vx, ox, gx, ux, vgx = _stream_proj(hx, w_in[layer], conv_w[layer], conv_b[layer], gate_bias[layer])
        qc, kc, vc, oc, gc, uc, vgc = _stream_proj(hc, w_in[layer], conv_w[layer], conv_b[layer], gate_bias[layer])
        zero = _zero_state(bsz)
        if last:
            fin_f, fin_b = _mlstm_bidir_final(kc, vc, gc, zero)
        else:
            hm_c, fin_f, fin_b = _mlstm_bidir(qc, kc, vc, gc, zero, zero)
        hm_x, _, _ = _mlstm_bidir(qx, kx, vx, gx, fin_f, fin_b)
        y = _mixer_out(hm_x, ox, ux, vgx, mlstm_norm_w[layer], cmlp_norm_w[layer], w_s[layer], b_s[layer], w_out[layer])
        x = _layer_norm(DEEPNORM_ALPHA * x + mod_x[:, 2] * y, ln1_w[layer], ln1_b[layer])
        y = _hier_moe(x * (1 + mod_x[:, 4]) + mod_x[:, 3], router1_w[layer], router1_b[layer],
                      router2_w[layer], router2_b[layer], w_gate[layer], w_up[layer], w_down[layer])
        x = _layer_norm(DEEPNORM_ALPHA * x + mod_x[:, 5] * y, ln2_w[layer], ln2_b[layer])
        if not last:
            yc = _mixer_out(hm_c, oc, uc, vgc, mlstm_norm_w[layer], cmlp_norm_w[layer], w_s[layer], b_s[layer], w_out[layer])
            ctx = _layer_norm(DEEPNORM_ALPHA * ctx + mod_c[2] * yc, ln1_w[layer], ln1_b[layer])
            yc = _hier_moe(ctx * (1 + mod_c[4]) + mod_c[3], router1_w[layer], router1_b[layer],
                           router2_w[layer], router2_b[layer], w_gate[layer], w_up[layer], w_down[layer])
            ctx = _layer_norm(DEEPNORM_ALPHA * ctx + mod_c[5] * yc, ln2_w[layer], ln2_b[layer])
    return x
```

```python
import os
import contextlib
import numpy as np
import concourse.bass as bass
import concourse.mybir as mybir
from concourse.bass_utils import run_bass_kernel_spmd

F32 = mybir.dt.float32
BF16 = mybir.dt.bfloat16
I32 = mybir.dt.int32
AF = mybir.ActivationFunctionType
ALU = mybir.AluOpType
AX = mybir.AxisListType

D = 2048
NT = 16
NTT = 18
NTOK = 2304
CAP = 512
NE = 32
NS = NE * CAP
ALPHA = 2.0 ** 0.25
EPS = 1e-6
NEG = -30000.0


class Tok:
    __slots__ = ("sem", "val", "eng")

    def __init__(self, sem, val, eng=None):
        self.sem = sem
        self.val = val
        self.eng = eng


class T:
    def __init__(self, t, name="", dram=False):
        self.t = t
        self.w = None
        self.r = []
        self.name = name
        self.dram = dram

    def __getitem__(self, idx):
        return self.t[idx]


class Eng:
    def __init__(self, name, h, sem, same_wait=True):
        self.name = name
        self.h = h
        self.sem = sem
        self.count = 0
        self.waited = {}
        self.pending = []
        self.last_ins = None
        self.last_has_inc = True
        self.same_wait = same_wait

    def flush(self):
        if not self.pending:
            return
        if not self.last_has_inc:
            self.count += 1
            self.last_ins.then_inc(self.sem, 1)
            self.last_has_inc = True
        for tk in self.pending:
            tk.val = self.count
        self.pending = []

    def wait(self, tok):
        if tok is None:
            return
        if tok.val is None:
            tok.eng.flush()
        if tok.sem is self.sem and not self.same_wait:
            return
        key = id(tok.sem)
        if self.waited.get(key, 0) >= tok.val:
            return
        self.h.wait_ge(tok.sem, tok.val)
        self.waited[key] = tok.val

    def emit(self, build, r=(), w=(), inc=True):
        for t in r:
            self.wait(t.w)
        for t in w:
            self.wait(t.w)
            for rr in t.r:
                self.wait(rr)
        ins = build()
        self.last_ins = ins
        if inc:
            self.count += 1
            ins.then_inc(self.sem, 1)
            self.last_has_inc = True
            tok = Tok(self.sem, self.count, self)
            for tk in self.pending:
                tk.val = self.count
            self.pending = []
        else:
            self.last_has_inc = False
            tok = Tok(self.sem, None, self)
            self.pending.append(tok)
        for t in r:
            t.r.append(tok)
        for t in w:
            t.w = tok
            t.r = []
        return tok


class FW:
    def __init__(self, nc, stack):
        self.nc = nc
        self.stack = stack
        mk = lambda n: stack.enter_context(nc.semaphore(n))
        self.pe = Eng("pe", nc.tensor, mk("s_pe"), same_wait=False)
        self.act = Eng("act", nc.scalar, mk("s_act"))
        self.dve = Eng("dve", nc.vector, mk("s_dve"))
        self.pool = Eng("pool", nc.gpsimd, mk("s_pool"))
        self.sp = Eng("sp", nc.sync, mk("s_sp"))
        self.engs = [self.pe, self.act, self.dve, self.pool, self.sp]
        self.dsems = {}
        self.dcount = {}

    def dsem(self, name):
        if name not in self.dsems:
            self.dsems[name] = self.stack.enter_context(self.nc.semaphore("d_" + name))
            self.dcount[name] = 0
        return self.dsems[name]

    def dma(self, q, out_t, out_ap, in_t, in_ap, sem=None, idx_t=None, **kw):
        sbt = out_t if (out_t is not None and not out_t.dram) else in_t
        if sem is None or not sem.startswith("grp_"):
            sem = "t_" + sbt.name
        s = self.dsem(sem)
        if in_t is not None:
            q.wait(in_t.w)
        if out_t is not None:
            q.wait(out_t.w)
            for rr in out_t.r:
                q.wait(rr)
        if idx_t is not None:
            q.wait(idx_t.w)
            ins = q.h.indirect_dma_start(out=out_ap, in_=in_ap, **kw)
        else:
            ins = q.h.dma_start(out=out_ap, in_=in_ap, **kw)
        self.dcount[sem] += 16
        ins.then_inc(s, 16)
        tok = Tok(s, self.dcount[sem])
        if in_t is not None:
            in_t.r.append(tok)
        if idx_t is not None:
            idx_t.r.append(tok)
        if out_t is not None:
            out_t.w = tok
            out_t.r = []
        return tok

    def group_done(self, sem, tiles):
        tok = Tok(self.dsems[sem], self.dcount[sem])
        for t in tiles:
            t.w = tok

    def barrier(self):
        for e in self.engs:
            e.flush()
        toks = [Tok(e.sem, e.count) for e in self.engs if e.count > 0]
        toks += [Tok(self.dsems[n], self.dcount[n]) for n in self.dsems if self.dcount[n] > 0]
        for e in self.engs:
            for tk in toks:
                if tk.sem is e.sem and not e.same_wait:
                    continue
                e.wait(tk)


def build(stage=99, dbg=None):
    nc = bass.Bass("TRN2", target_bir_lowering=False)
    names = {}

    def din(name, shape, dt=F32):
        names[name] = nc.dram_tensor(name, list(shape), dt, kind="ExternalInput").ap()
        return names[name]

    x_d = din("x", [2048, D])
    ctx_d = din("ctx", [256, D])
    pos_d = din("pos", [2048, D])
    ccT_d = din("ccT", [128, 16, 2])
    wmod_d = din("w_mod", [D, 6 * D])
    bmod_d = din("b_mod", [6 * D])
    win_d = din("w_in", [D, 6160])
    cwT_d = din("conv_wT", [128, 3, 16])
    cbT_d = din("conv_bT", [128, 16])
    gb_d = din("gate_bias", [16])
    mnw_d = din("mlstm_norm_w", [1024])
    cnw_d = din("cmlp_norm_w", [1024])
    wsT_d = din("w_sT", [4, 128, 128])
    bsT_d = din("b_sT", [128, 4])
    ident_d = din("ident", [128, 128])
    triF_d = din("triF", [128, 128])
    triB_d = din("triB", [128, 128])
    triS_d = din("triS", [128, 128])
    mF_d = din("maskF", [128, 128])
    mB_d = din("maskB", [128, 128])
    if stage >= 4:
        wout_d = din("w_out", [D, D])
        ln1w_d = din("ln1_w", [D])
        ln1b_d = din("ln1_b", [D])
        rw_d = din("rw", [D, 36])
        rb_d = din("rb", [36])
        ecap_d = din("ecap", [32])
    if stage >= 5:
        wg_d = din("w_gate", [NE, D, 1024])
        wu_d = din("w_up", [NE, D, 1024])
        wd_d = din("w_down", [NE, 1024, D])
        ln2w_d = din("ln2_w", [D])
        ln2b_d = din("ln2_b", [D])
        out_d = nc.dram_tensor("out", [2048, D], F32, kind="ExternalOutput").ap()

    mod_d = nc.dram_tensor("mod_scr", [2, 6 * D], F32, kind="Internal").ap()
    y_d = nc.dram_tensor("y_scr", [2048, D], BF16, kind="Internal").ap()
    x1_d = nc.dram_tensor("x1_scr", [2048, D], F32, kind="Internal").ap()
    xs_d = nc.dram_tensor("xs_scr", [NS + 128, D], BF16, kind="Internal").ap()
    ys_d = nc.dram_tensor("ys_scr", [NS + 128, D], BF16, kind="Internal").ap()
    MOD, YD, X1D, XSD, YSD = [T(a_, n_, dram=True) for a_, n_ in ((mod_d, "MOD"), (y_d, "YD"), (x1_d, "X1D"), (xs_d, "XSD"), (ys_d, "YSD"))]
    dbg_out = {}

    def dout(name, shape, dt=F32):
        dbg_out[name] = nc.dram_tensor(name, list(shape), dt, kind="ExternalOutput").ap()
        return dbg_out[name]

    with contextlib.ExitStack() as top:
        fw = FW(nc, top)
        pe, act, dve, pool, sp = fw.pe, fw.act, fw.dve, fw.pool, fw.sp

        uid = [0]

        def mk(st, kind):
            def f(name, shape, dt=F32):
                a = nc.sbuf_tensor if kind == "sb" else nc.psum_tensor
                uid[0] += 1
                return T(st.enter_context(a("%s_%s%d" % (kind, name, uid[0]), list(shape), dt)), name)
            return f

        sbG = mk(top, "sb")
        ident = sbG("ident", [128, 128])
        identb = sbG("identb", [128, 128], BF16)
        triF = sbG("triF", [128, 128])
        triB = sbG("triB", [128, 128])
        triS = sbG("triS", [128, 128])
        mF = sbG("mF", [128, 128])
        mB = sbG("mB", [128, 128])
        ones = sbG("ones", [128, 128])
        for t_, d_ in ((ident, ident_d), (triF, triF_d), (triB, triB_d), (triS, triS_d), (mF, mF_d), (mB, mB_d)):
            fw.dma(sp, t_, t_[:], None, d_, sem="grp_c0")
        fw.group_done("grp_c0", [ident, triF, triB, triS, mF, mB])
        dve.emit(lambda: nc.vector.tensor_copy(out=identb[:], in_=ident[:]), r=[ident], w=[identb])
        dve.emit(lambda: nc.vector.memset(ones[:], 1.0), w=[ones])
        sc1 = sbG("sc1", [128, 4, 16])

        with contextlib.ExitStack() as ph:
            sb, psm = mk(ph, "sb"), mk(ph, "ps")
            cc = sb("cc", [128, 16, 2])
            sil = sb("sil", [128, 16, 2], BF16)
            bm = sb("bm", [2, 6 * D])
            modsb = sb("modsb", [2, 6 * D])
            wm = [sb("wm%d" % i, [128, 16, 1536], BF16) for i in range(2)]
            pm = [psm("pm%d" % i, [2, 3, 512]) for i in range(2)]
            fw.dma(sp, cc, cc[:], None, ccT_d)
            fw.dma(sp, bm, bm[:], None, bmod_d.partition_broadcast(2))
            act.emit(lambda: nc.scalar.activation(out=sil[:], in_=cc[:], func=AF.Silu), r=[cc], w=[sil])
            wv = wmod_d.rearrange("(kc p) n -> p kc n", p=128)
            for cg in range(8):
                w_ = wm[cg % 2]
                for hh in range(2):
                    fw.dma(pool, w_, w_[:, hh * 8:(hh + 1) * 8, :], None,
                           wv[:, hh * 8:(hh + 1) * 8, cg * 1536:(cg + 1) * 1536], sem="wld")
                p_ = pm[cg % 2]
                for j in range(3):
                    for kc in range(16):
                        pe.emit(lambda: nc.tensor.matmul(p_[0:2, j, :], lhsT=sil[:, kc, :], rhs=w_[:, kc, j * 512:(j + 1) * 512],
                                                         start=(kc == 0), stop=(kc == 15)),
                                r=[sil, w_], w=[p_], inc=(j == 2 and kc == 15))
                dve.emit(lambda: nc.vector.tensor_tensor(out=modsb[0:2, cg * 1536:(cg + 1) * 1536],
                                                         in0=p_[0:2, :, :].rearrange("p a b -> p (a b)"),
                                                         in1=bm[0:2, cg * 1536:(cg + 1) * 1536], op=ALU.add),
                         r=[p_, bm], w=[modsb])
            fw.dma(sp, MOD, mod_d, modsb, modsb[0:2, :], sem="st")
            t16 = sb("t16", [16, 4, 128])
            pt16 = psm("pt16", [128, 4, 16])
            for v, (row, n) in enumerate(((0, 0), (0, 1), (1, 0), (1, 1))):
                fw.dma(sp, t16, t16[0:16, v, :], MOD, mod_d[row, n * D:(n + 1) * D].rearrange("(kc p) -> kc p", p=128))
            for v in range(4):
                pe.emit(lambda: nc.tensor.transpose(out=pt16[:, v, :], in_=t16[0:16, v, :], identity=ident[0:16, 0:16]),
                        r=[t16, ident], w=[pt16], inc=(v == 3))
            dve.emit(lambda: nc.vector.tensor_copy(out=sc1[:], in_=pt16[:]), r=[pt16], w=[sc1])
            for v in (1, 3):
                dve.emit(lambda: nc.vector.tensor_scalar(out=sc1[:, v, :], in0=sc1[:, v, :], scalar1=1.0, scalar2=None, op0=ALU.add),
                         r=[sc1], w=[sc1])
            if stage == 1:
                o = dout("dbg_mod", [2, 6 * D])
                fw.dma(sp, None, o, modsb, modsb[0:2, :], sem="st")
                o2 = dout("dbg_sc1", [128, 64])
                fw.dma(sp, None, o2, sc1, sc1[:].rearrange("p a b -> p (a b)"), sem="st")
            fw.barrier()
        if stage == 1:
            return nc, names, dbg_out

        with contextlib.ExitStack() as ph:
            sb, psm = mk(ph, "sb"), mk(ph, "ps")
            hxT = sb("hxT", [128, 16, NTOK], BF16)
            with contextlib.ExitStack() as p2:
                sb2, ps2 = mk(p2, "sb"), mk(p2, "ps")
                xt = [sb2("xt%d" % i, [128, D]) for i in range(2)]
                ptl = [sb2("ptl%d" % i, [128, D]) for i in range(2)]
                ptr = [ps2("ptr%d" % i, [128, 4, 128]) for i in range(3)]
                n_tr = 0
                for i in range(NTT):
                    a, b_ = xt[i % 2], ptl[i % 2]
                    if i < NT:
                        fw.dma(sp, a, a[:], None, x_d[i * 128:(i + 1) * 128, :])
                        fw.dma(sp, b_, b_[:], None, pos_d[i * 128:(i + 1) * 128, :])
                        pool.emit(lambda: nc.gpsimd.tensor_tensor(out=a[:], in0=a[:], in1=b_[:], op=ALU.add), r=[b_, a], w=[a])
                        sv = 0
                    else:
                        fw.dma(sp, a, a[:], None, ctx_d[(i - NT) * 128:(i - NT + 1) * 128, :])
                        sv = 2
                    tk0 = NT * 128 + (i - NT) * 128 if i >= NT else i * 128
                    for g4 in range(4):
                        p_ = ptr[n_tr % 3]
                        n_tr += 1
                        for q4 in range(4):
                            kc = g4 * 4 + q4
                            pe.emit(lambda: nc.tensor.transpose(out=p_[:, q4, :], in_=a[:, kc * 128:(kc + 1) * 128], identity=ident[:]),
                                    r=[a, ident], w=[p_], inc=(q4 == 3))
                        for q4 in range(4):
                            kc = g4 * 4 + q4
                            if q4 % 2 == 0:
                                dve.emit(lambda: nc.vector.tensor_scalar(out=hxT[:, kc, tk0:tk0 + 128], in0=p_[:, q4, :],
                                                                         scalar1=sc1[:, sv + 1, kc:kc + 1], scalar2=sc1[:, sv, kc:kc + 1],
                                                                         op0=ALU.mult, op1=ALU.add), r=[p_, sc1], w=[hxT])
                            else:
                                act.emit(lambda: nc.scalar.activation(out=hxT[:, kc, tk0:tk0 + 128], in_=p_[:, q4, :], func=AF.Identity,
                                                                      scale=sc1[:, sv + 1, kc:kc + 1], bias=sc1[:, sv, kc:kc + 1]),
                                         r=[p_, sc1], w=[hxT])
                fw.barrier()
            if stage == 2:
                o = dout("dbg_hxT", [128, 16 * NTOK], BF16)
                fw.dma(sp, None, o, hxT, hxT[:].rearrange("p a b -> p (a b)"), sem="st")
                fw.barrier()
                return nc, names, dbg_out

            wsl = [sb("wsl%d" % i, [128, 16, 256], BF16) for i in range(4)]
            wsl_n = [0]
            winv = win_d.rearrange("(kc p) n -> p kc n", p=128)

            def load_w(c0, ncol=256):
                s_ = wsl[wsl_n[0] % 4]
                wsl_n[0] += 1
                fw.dma(pool, s_, s_[:, :, 0:ncol], None, winv[:, :, c0:c0 + ncol], sem="wld")
                return s_

            G = sb("G", [128, NTT, 16])
            CS = sb("CS", [128, NTT, 3, 16])
            CB8 = sb("CB8", [128, NTT, 8])
            EB8 = sb("EB8", [128, NTT, 8])
            TOT8 = sb("TOT8", [128, NTT, 8])
            W8 = sb("W8", [128, NTT, 8])
            EL8 = sb("EL8", [128, NTT, 8])
            gbc = sb("gbc", [128, 16])
            cw = sb("cw", [128, 3, 16])
            cb = sb("cb", [128, 16])
            mnw = sb("mnw", [128, 1024])
            cnw = sb("cnw", [128, 1024])
            bsT = sb("bsT", [128, 4])
            fw.dma(sp, gbc, gbc[:], None, gb_d.partition_broadcast(128), sem="grp_c3")
            fw.dma(sp, cw, cw[:], None, cwT_d, sem="grp_c3")
            fw.dma(sp, cb, cb[:], None, cbT_d, sem="grp_c3")
            fw.dma(sp, mnw, mnw[:], None, mnw_d.partition_broadcast(128), sem="grp_c3")
            fw.dma(sp, cnw, cnw[:], None, cnw_d.partition_broadcast(128), sem="grp_c3")
            fw.dma(sp, bsT, bsT[:], None, bsT_d, sem="grp_c3")
            fw.group_done("grp_c3", [gbc, cw, cb, mnw, cnw, bsT])

            pbig_t = [ph.enter_context(nc.psum_tensor("ps_pbig%d" % i, [128, 512], F32)) for i in range(2)]
            pbig = [T(t_) for t_ in pbig_t]
            pg = [T(t_[:, 0:48].rearrange("p (a b) -> p a b", a=3)) for t_ in pbig_t]
            for k_ in range(2):
                pg[k_] = pbig[k_]
            pgv = [t_[:, 0:48].rearrange("p (a b) -> p a b", a=3) for t_ in pbig_t]
            wg_ = load_w(4096, 16)
            for i in range(NTT):
                p_ = pg[i % 2]
                pv_ = pgv[i % 2]
                for kc in range(16):
                    pe.emit(lambda: nc.tensor.matmul(pv_[:, 0, :], lhsT=hxT[:, kc, i * 128:(i + 1) * 128], rhs=wg_[:, kc, 0:16],
                                                     start=(kc == 0), stop=(kc == 15)), r=[hxT, wg_], w=[p_], inc=(kc == 15))
                dve.emit(lambda: nc.vector.tensor_tensor(out=G[:, i, :], in0=pv_[:, 0, :], in1=gbc[:], op=ALU.add), r=[p_, gbc], w=[G])
            def cutG(nm):
                if stage == 3 and dbg == nm:
                    o = dout("dbg_G", [128, NTT * 16])
                    fw.dma(sp, None, o, G, G[:].rearrange("p a b -> p (a b)"), sem="st")
                    fw.barrier()
                    return True
                return False
            def cut(nm, t_, ap_, shape, dt=F32):
                if stage == 3 and dbg == nm:
                    o = dout("dbg_" + nm, shape, dt)
                    fw.dma(sp, None, o, t_, ap_, sem="st")
                    fw.barrier()
                    return True
                return False
            if cutG("g1"):
                return nc, names, dbg_out
            for c0 in (4, 12):
                v_ = G[:, :, c0:c0 + 4]
                act.emit(lambda: nc.scalar.activation(out=v_, in_=v_, func=AF.Exp, scale=-1.0), r=[G], w=[G])
                act.emit(lambda: nc.scalar.activation(out=v_, in_=v_, func=AF.Ln, bias=1.0, scale=1.0), r=[G], w=[G])
                dve.emit(lambda: nc.vector.tensor_scalar(out=v_, in0=v_, scalar1=-1.0, scalar2=None, op0=ALU.mult), r=[G], w=[G])
            if cutG("g2"):
                return nc, names, dbg_out
            for i in range(NTT):
                p_ = pg[i % 2]
                pv_ = pgv[i % 2]
                for j, l_ in enumerate((triF, triB, ones)):
                    pe.emit(lambda: nc.tensor.matmul(pv_[:, j, :], lhsT=l_[:], rhs=G[:, i, :], start=True, stop=True),
                            r=[l_, G], w=[p_], inc=(j == 2))
                dve.emit(lambda: nc.vector.tensor_copy(out=CS[:, i, :, :], in_=pv_), r=[p_], w=[CS])
            if cutG("g3"):
                return nc, names, dbg_out
            dve.emit(lambda: nc.vector.tensor_tensor(out=CB8[:, :, 0:4], in0=G[:, :, 0:4], in1=CS[:, :, 0, 4:8], op=ALU.subtract), r=[G, CS], w=[CB8])
            dve.emit(lambda: nc.vector.tensor_tensor(out=CB8[:, :, 4:8], in0=G[:, :, 8:12], in1=CS[:, :, 1, 12:16], op=ALU.subtract), r=[G, CS], w=[CB8])
            dve.emit(lambda: nc.vector.tensor_copy(out=TOT8[:, :, 0:4], in_=CS[:, :, 2, 4:8]), r=[CS], w=[TOT8])
            dve.emit(lambda: nc.vector.tensor_copy(out=TOT8[:, :, 4:8], in_=CS[:, :, 2, 12:16]), r=[CS], w=[TOT8])
            if cutG("g4"):
                return nc, names, dbg_out
            act.emit(lambda: nc.scalar.activation(out=EB8[:, :, 0:4], in_=CS[:, :, 0, 4:8], func=AF.Exp), r=[CS], w=[EB8])
            act.emit(lambda: nc.scalar.activation(out=EB8[:, :, 4:8], in_=CS[:, :, 1, 12:16], func=AF.Exp), r=[CS], w=[EB8])
            if cutG("g5"):
                return nc, names, dbg_out
            dve.emit(lambda: nc.vector.tensor_tensor(out=W8[:], in0=CB8[:], in1=TOT8[:], op=ALU.add), r=[CB8, TOT8], w=[W8])
            act.emit(lambda: nc.scalar.activation(out=W8[:], in_=W8[:], func=AF.Exp), r=[W8], w=[W8])
            act.emit(lambda: nc.scalar.activation(out=EL8[:], in_=TOT8[:], func=AF.Exp), r=[TOT8], w=[EL8])
            if cutG("g6"):
                return nc, names, dbg_out
            if stage == 3 and dbg == "gates":
                o = dout("dbg_G", [128, NTT * 16])
                fw.dma(sp, None, o, G, G[:].rearrange("p a b -> p (a b)"), sem="st")
                o = dout("dbg_CS", [128, NTT * 48])
                fw.dma(sp, None, o, CS, CS[:].rearrange("p a b c -> p (a b c)"), sem="st")
                fw.barrier()
                return nc, names, dbg_out

            QT = sb("QT", [128, 2, NTOK], BF16)
            KT = sb("KT", [128, 2, NTOK], BF16)
            Kt = sb("Kt", [128, NTT, 256], BF16)
            V1 = sb("V1", [128, NTT, 258], BF16)
            SO = sb("SO", [128, NT, 256], BF16)
            SCR = sb("SCR", [128, 2, NTOK])
            class _V:
                def __init__(s_, par, fn):
                    s_.par, s_.fn = par, fn
                def __getitem__(s_, idx):
                    return s_.fn(s_.par.t)[idx]
            P32v = _V(SCR, lambda t_: t_[:, 0, :])
            C32v = _V(SCR, lambda t_: t_[:, 1, :])
            HFv = _V(SCR, lambda t_: t_[:].rearrange("p a b -> p (a b)")[:, 0:NT * 256].rearrange("p (a b) -> p a b", a=NT))
            CT32 = sb("CT32", [128, 2, 257])
            CTb = sb("CTb", [128, 2, 258], BF16)
            AT = [sb("AT%d" % i, [128, 128]) for i in range(2)]
            PT = [sb("PT%d" % i, [128, 128], BF16) for i in range(2)]
            TA = [sb("TA%d" % i, [128, 257]) for i in range(2)]
            NN = [sb("NN%d" % i, [128, 257]) for i in range(2)]
            WV = [sb("WV%d" % i, [128, 258], BF16) for i in range(2)]
            HH = [sb("HH%d" % i, [128, 256]) for i in range(2)]
            YM = [sb("YM%d" % i, [128, 256], BF16) for i in range(2)]
            sm = [sb("sm%d" % i, [128, 16]) for i in range(2)]
            bst = [sb("bst%d" % i, [128, 6]) for i in range(2)]
            pSB_t = ph.enter_context(nc.psum_tensor("ps_pSB", [128, 512], F32))
            pSB = T(pSB_t)
            pSTv = pSB_t[:, 0:128]
            pBRv = pSB_t[:, 128:256]
            pNA = psm("pNA", [128, 512])
            pNB = psm("pNB", [128, 512])
            pCL = psm("pCL", [128, 2, 512])
            pKb = psm("pKb", [128, 4, 2, 128], BF16)
            dve.emit(lambda: nc.vector.memset(V1[:], 1.0), w=[V1])
            n_big = [0]
            n_it = [0]

            def ln_rows(src_t, src_ap, k):
                s_ = sm[k]
                b_ = bst[k]
                dve.emit(lambda: nc.vector.bn_stats(out=b_[:], in_=src_ap), r=[src_t], w=[b_])
                dve.emit(lambda: nc.vector.bn_aggr(out=s_[:, 0:2], in_=b_[:]), r=[b_], w=[s_])
                dve.emit(lambda: nc.vector.tensor_scalar(out=s_[:, 2:3], in0=s_[:, 1:2], scalar1=EPS, scalar2=None, op0=ALU.add), r=[s_], w=[s_])
                act.emit(lambda: nc.scalar.activation(out=s_[:, 3:4], in_=s_[:, 2:3], func=AF.Sqrt), r=[s_], w=[s_])
                dve.emit(lambda: nc.vector.reciprocal(out=s_[:, 4:5], in_=s_[:, 3:4]), r=[s_], w=[s_])
                dve.emit(lambda: nc.vector.scalar_tensor_tensor(out=s_[:, 5:6], in0=s_[:, 0:1], scalar=-1.0, in1=s_[:, 4:5],
                                                                op0=ALU.mult, op1=ALU.mult), r=[s_], w=[s_])
                return s_[:, 4:5], s_[:, 5:6]

            tgroups = [(0, 512), (512, 512), (1024, 512), (1536, 512), (2048, 256)]
            for h in range(4 if stage >= 3 else 0):
                sq = load_w(h * 256)
                sk = load_w(1024 + h * 256)
                sv_ = load_w(2048 + h * 256)
                so_ = load_w(3072 + h * 256)
                for isk, (s_, dst) in enumerate(((sq, QT), (sk, KT))):
                    for ec in range(2):
                        for (t0, n) in tgroups:
                            p_ = pbig[n_big[0] % 2]
                            n_big[0] += 1
                            for kc in range(16):
                                pe.emit(lambda: nc.tensor.matmul(p_[:, 0:n], lhsT=s_[:, kc, ec * 128:(ec + 1) * 128], rhs=hxT[:, kc, t0:t0 + n],
                                                                 start=(kc == 0), stop=(kc == 15)), r=[s_, hxT], w=[p_], inc=(kc == 15))
                            act.emit(lambda: nc.scalar.copy(out=P32v[:, t0:t0 + n], in_=p_[:, 0:n]), r=[p_], w=[SCR])
                        ci = isk * 8 + h * 2 + ec
                        dve.emit(lambda: nc.vector.tensor_scalar(out=C32v[:], in0=P32v[:], scalar1=cw[:, 1, ci:ci + 1], scalar2=cb[:, ci:ci + 1],
                                                                 op0=ALU.mult, op1=ALU.add), r=[SCR, cw, cb], w=[SCR])
                        for (a0, a1) in ((0, 2048), (2048, 2304)):
                            dve.emit(lambda: nc.vector.scalar_tensor_tensor(out=C32v[:, a0 + 1:a1], in0=P32v[:, a0:a1 - 1], scalar=cw[:, 0, ci:ci + 1],
                                                                            in1=C32v[:, a0 + 1:a1], op0=ALU.mult, op1=ALU.add), r=[SCR, cw], w=[SCR])
                            dve.emit(lambda: nc.vector.scalar_tensor_tensor(out=C32v[:, a0:a1 - 1], in0=P32v[:, a0 + 1:a1], scalar=cw[:, 2, ci:ci + 1],
                                                                            in1=C32v[:, a0:a1 - 1], op0=ALU.mult, op1=ALU.add), r=[SCR, cw], w=[SCR])
                        if isk == 0:
                            act.emit(lambda: nc.scalar.activation(out=QT[:, ec, :], in_=C32v[:], func=AF.Silu), r=[SCR], w=[QT])
                        else:
                            act.emit(lambda: nc.scalar.activation(out=C32v[:], in_=C32v[:], func=AF.Silu), r=[SCR], w=[SCR])
                            pool.emit(lambda: nc.gpsimd.tensor_scalar(out=KT[:, ec, :], in0=C32v[:], scalar1=0.0625, scalar2=None, op0=ALU.mult),
                                      r=[SCR], w=[KT])
                if cut("h1", QT, QT[:].rearrange("p a b -> p (a b)"), [128, 2 * NTOK], BF16):
                    return nc, names, dbg_out
                if cut("h1k", KT, KT[:].rearrange("p a b -> p (a b)"), [128, 2 * NTOK], BF16):
                    return nc, names, dbg_out
                for i in range(NTT):
                    pb = pbig[n_big[0] % 2]
                    n_big[0] += 1
                    for kc in range(16):
                        pe.emit(lambda: nc.tensor.matmul(pb[:, 0:256], lhsT=hxT[:, kc, i * 128:(i + 1) * 128], rhs=sv_[:, kc, :],
                                                         start=(kc == 0), stop=(kc == 15)), r=[hxT, sv_], w=[pb], inc=(kc == 15))
                    if i < NT:
                        po_ = (pNA, pNB)[i % 2]
                        for kc in range(16):
                            pe.emit(lambda: nc.tensor.matmul(po_[:, 0:256], lhsT=hxT[:, kc, i * 128:(i + 1) * 128], rhs=so_[:, kc, :],
                                                             start=(kc == 0), stop=(kc == 15)), r=[hxT, so_], w=[po_], inc=(kc == 15))
                        act.emit(lambda: nc.scalar.activation(out=SO[:, i, :], in_=po_[:, 0:256], func=AF.Sigmoid), r=[po_], w=[SO])
                    if "v" not in os.environ.get("KSKIP", ""):
                        dve.emit(lambda: nc.vector.tensor_copy(out=V1[:, i, 0:256], in_=pb[:, 0:256]), r=[pb], w=[V1])
                if cut("h2a", SO, SO[:].rearrange("p a b -> p (a b)"), [128, NT * 256], BF16):
                    return nc, names, dbg_out
                for i0 in range(0, NTT, 4):
                    nt4 = min(4, NTT - i0)
                    for j4 in range(nt4):
                        i = i0 + j4
                        for ec in range(2):
                            pe.emit(lambda: nc.tensor.transpose(out=pKb[:, j4, ec, :], in_=KT[:, ec, i * 128:(i + 1) * 128], identity=identb[:]),
                                    r=[KT, identb], w=[pKb], inc=(ec == 1 and j4 == nt4 - 1))
                    act.emit(lambda: nc.scalar.copy(out=Kt[:, i0:i0 + nt4, :], in_=pKb[:, 0:nt4, :, :].rearrange("p a b c -> p a (b c)")), r=[pKb], w=[Kt])

                if cut("h2", V1, V1[:].rearrange("p a b -> p (a b)"), [128, NTT * 258], BF16):
                    return nc, names, dbg_out
                if cut("h2k", Kt, Kt[:].rearrange("p a b -> p (a b)"), [128, NTT * 256], BF16):
                    return nc, names, dbg_out
                for d_ in range(2):
                    order = [16, 17] + list(range(16)) if d_ == 0 else [17, 16] + list(range(15, -1, -1))
                    c8 = h if d_ == 0 else 4 + h
                    gl = 4 + h if d_ == 0 else 12 + h
                    tri_, msk_ = (triF, mF) if d_ == 0 else (triB, mB)
                    dve.emit(lambda: nc.vector.memset(CT32[:], 0.0), w=[CT32])
                    dve.emit(lambda: nc.vector.memset(CTb[:], 0.0), w=[CTb])
                    if d_ == 1 and cut("h3", SCR, HFv[:, :, :], [128, NT, 256]):
                        return nc, names, dbg_out
                    for oi, i in enumerate(order):
                        k2 = n_it[0] % 2
                        n_it[0] += 1
                        tsl = slice(i * 128, (i + 1) * 128)
                        if i < NT:
                            for ec in range(2):
                                pe.emit(lambda: nc.tensor.matmul(pSTv, lhsT=KT[:, ec, tsl], rhs=QT[:, ec, tsl], start=(ec == 0), stop=(ec == 1)),
                                        r=[KT, QT], w=[pSB], inc=False)
                            pe.emit(lambda: nc.tensor.matmul(pBRv, lhsT=G[:, i, gl:gl + 1].to_broadcast([128, 128]), rhs=tri_[:], start=True, stop=False),
                                    r=[G, tri_], w=[pSB], inc=False)
                            pe.emit(lambda: nc.tensor.matmul(pBRv, lhsT=ident[:], rhs=msk_[:], start=False, stop=True),
                                    r=[ident, msk_], w=[pSB])
                            at_, pt_, ta_, nn_ = AT[k2], PT[k2], TA[k2], NN[k2]
                            act.emit(lambda: nc.scalar.activation(out=at_[:], in_=pBRv, func=AF.Exp, bias=CB8[:, i, c8:c8 + 1], scale=1.0),
                                     r=[pSB, CB8], w=[at_])
                            dve.emit(lambda: nc.vector.tensor_tensor(out=pt_[:], in0=at_[:], in1=pSTv, op=ALU.mult), r=[at_, pSB], w=[pt_])
                            for ec in range(2):
                                pe.emit(lambda: nc.tensor.matmul(pNA[:, 0:257], lhsT=QT[:, ec, tsl], rhs=CTb[:, ec, 0:257], start=(ec == 0), stop=(ec == 1)),
                                        r=[QT, CTb], w=[pNA], inc=(ec == 1))
                            pe.emit(lambda: nc.tensor.matmul(pNB[:, 0:257], lhsT=pt_[:], rhs=V1[:, i, 0:257], start=True, stop=True), r=[pt_, V1], w=[pNB])
                            act.emit(lambda: nc.scalar.activation(out=ta_[:], in_=pNA[:, 0:257], func=AF.Copy, scale=EB8[:, i, c8:c8 + 1]),
                                     r=[pNA, EB8], w=[ta_])
                            dve.emit(lambda: nc.vector.tensor_tensor(out=nn_[:], in0=ta_[:], in1=pNB[:, 0:257], op=ALU.add), r=[ta_, pNB], w=[nn_])
                            s_ = sm[k2]
                            dve.emit(lambda: nc.vector.tensor_scalar(out=s_[:, 10:11], in0=nn_[:, 256:257], scalar1=-1.0, scalar2=1.0, op0=ALU.mult, op1=ALU.max), r=[nn_], w=[s_])
                            dve.emit(lambda: nc.vector.tensor_tensor(out=s_[:, 8:9], in0=nn_[:, 256:257], in1=s_[:, 10:11], op=ALU.max), r=[nn_, s_], w=[s_])
                            dve.emit(lambda: nc.vector.reciprocal(out=s_[:, 9:10], in_=s_[:, 8:9]), r=[s_], w=[s_])
                            if d_ == 0:
                                dve.emit(lambda: nc.vector.tensor_scalar(out=HFv[:, i, :], in0=nn_[:, 0:256], scalar1=s_[:, 9:10], scalar2=None, op0=ALU.mult),
                                         r=[nn_, s_], w=[SCR])
                            else:
                                hh_, ym_ = HH[k2], YM[k2]
                                dve.emit(lambda: nc.vector.scalar_tensor_tensor(out=hh_[:], in0=nn_[:, 0:256], scalar=s_[:, 9:10], in1=HFv[:, i, :],
                                                                                op0=ALU.mult, op1=ALU.add), r=[nn_, s_, SCR], w=[hh_])
                                rs, nb = ln_rows(hh_, hh_[:], k2)
                                act.emit(lambda: nc.scalar.activation(out=hh_[:], in_=hh_[:], func=AF.Identity, scale=rs, bias=nb), r=[hh_, s_], w=[hh_])
                                pool.emit(lambda: nc.gpsimd.tensor_tensor(out=hh_[:], in0=hh_[:], in1=mnw[:, h * 256:(h + 1) * 256], op=ALU.mult),
                                          r=[hh_, mnw], w=[hh_])
                                pool.emit(lambda: nc.gpsimd.tensor_tensor(out=ym_[:], in0=hh_[:], in1=SO[:, i, :], op=ALU.mult), r=[hh_, SO], w=[ym_])
                                fw.dma(sp, YD, y_d[tsl, h * 256:(h + 1) * 256], ym_, ym_[:], sem="st")
                        if oi < NTT - 1:
                            wv_ = WV[k2]
                            pool.emit(lambda: nc.gpsimd.tensor_scalar(out=wv_[:, 0:257], in0=V1[:, i, 0:257], scalar1=W8[:, i, c8:c8 + 1], scalar2=None, op0=ALU.mult),
                                      r=[V1, W8], w=[wv_])
                            for ec in range(2):
                                pe.emit(lambda: nc.tensor.matmul(pCL[:, ec, 0:257], lhsT=Kt[:, i, ec * 128:(ec + 1) * 128], rhs=wv_[:, 0:257], start=True, stop=True),
                                        r=[Kt, wv_], w=[pCL], inc=(ec == 1))
                            dve.emit(lambda: nc.vector.scalar_tensor_tensor(out=CT32[:], in0=CT32[:], scalar=EL8[:, i, c8:c8 + 1], in1=pCL[:, :, 0:257],
                                                                            op0=ALU.mult, op1=ALU.add), r=[CT32, EL8, pCL], w=[CT32])
                            act.emit(lambda: nc.scalar.copy(out=CTb[:, :, 0:257], in_=CT32[:]), r=[CT32], w=[CTb])

            wsb = sb("wsb", [128, 4, 128], BF16)
            fw.dma(pool, wsb, wsb[:], None, wsT_d.rearrange("g q p -> q g p"), sem="wld")
            U = [sb("U%d" % i, [128, 256]) for i in range(2)]
            VG = [sb("VG%d" % i, [128, 256]) for i in range(2)]
            VN = [sb("VN%d" % i, [128, 256], BF16) for i in range(2)]
            for g in range(4 if stage >= 3 else 0):
                su = load_w(4112 + g * 256)
                sg = load_w(5136 + g * 256)
                for i in range(NT):
                    k2 = i % 2
                    pb = pbig[n_big[0] % 2]
                    n_big[0] += 1
                    tsl = slice(i * 128, (i + 1) * 128)
                    for jj, s_ in enumerate((su, sg)):
                        for kc in range(16):
                            pe.emit(lambda: nc.tensor.matmul(pb[:, jj * 256:(jj + 1) * 256], lhsT=hxT[:, kc, tsl], rhs=s_[:, kc, :],
                                                             start=(kc == 0), stop=(kc == 15)), r=[hxT, s_], w=[pb], inc=(kc == 15 and jj == 1))
                    u_, vg_, vn_, ym_ = U[k2], VG[k2], VN[k2], YM[k2]
                    act.emit(lambda: nc.scalar.activation(out=u_[:], in_=pb[:, 0:256], func=AF.Gelu_apprx_tanh), r=[pb], w=[u_])
                    act.emit(lambda: nc.scalar.activation(out=vg_[:], in_=pb[:, 256:512], func=AF.Gelu_apprx_tanh), r=[pb], w=[vg_])
                    rs, nb = ln_rows(vg_, vg_[:], k2)
                    act.emit(lambda: nc.scalar.activation(out=vg_[:], in_=vg_[:], func=AF.Identity, scale=rs, bias=nb), r=[vg_, sm[k2]], w=[vg_])
                    pool.emit(lambda: nc.gpsimd.tensor_tensor(out=vn_[:], in0=vg_[:], in1=cnw[:, g * 256:(g + 1) * 256], op=ALU.mult), r=[vg_, cnw], w=[vn_])
                    pe.emit(lambda: nc.tensor.matmul(pNA[:, 0:256], lhsT=wsb[:, g, :], rhs=vn_[:], start=True, stop=True), r=[wsb, vn_], w=[pNA])
                    dve.emit(lambda: nc.vector.scalar_tensor_tensor(out=ym_[:], in0=pNA[:, 0:256], scalar=bsT[:, g:g + 1], in1=u_[:],
                                                                    op0=ALU.add, op1=ALU.mult), r=[pNA, bsT, u_], w=[ym_])
                    fw.dma(sp, YD, y_d[tsl, 1024 + g * 256:1024 + (g + 1) * 256], ym_, ym_[:], sem="st")
            fw.barrier()
        if stage == 3:
            o = dout("dbg_y", [2048, D], BF16)
            with nc.sbuf_tensor("ytmp", [128, 16, D], BF16) as yt_:
                YT_ = T(yt_)
                fw.dma(sp, YT_, yt_[:], YD, y_d.rearrange("(a p) d -> p a d", p=128))
                fw.dma(sp, None, o.rearrange("(a p) d -> p a d", p=128), YT_, yt_[:], sem="st")
                fw.barrier()
            return nc, names, dbg_out

        SLT = sbG("SLT", [128, NT, 2], I32)
        GT = sbG("GT", [128, NT, 2])
        with contextlib.ExitStack() as ph:
            sb, psm = mk(ph, "sb"), mk(ph, "ps")
            wo = sb("wo", [128, 16, D], BF16)
            wov = wout_d.rearrange("(kc p) n -> p kc n", p=128)
            for q4 in range(4):
                fw.dma(pool, wo, wo[:, q4 * 4:(q4 + 1) * 4, :], None, wov[:, q4 * 4:(q4 + 1) * 4, :], sem="wld")
            g1b = sb("g1b", [128, D])
            l1w = sb("l1w", [128, D])
            l1b = sb("l1b", [128, D])
            A2 = sb("A2", [128, D])
            B2 = sb("B2", [128, D])
            fw.dma(sp, g1b, g1b[:], MOD, mod_d[0, 2 * D:3 * D].partition_broadcast(128))
            fw.dma(sp, l1w, l1w[:], None, ln1w_d.partition_broadcast(128))
            fw.dma(sp, l1b, l1b[:], None, ln1b_d.partition_broadcast(128))
            fw.dma(sp, A2, A2[:], MOD, mod_d[0, 4 * D:5 * D].partition_broadcast(128))
            fw.dma(sp, B2, B2[:], MOD, mod_d[0, 3 * D:4 * D].partition_broadcast(128))
            dve.emit(lambda: nc.vector.tensor_scalar(out=A2[:], in0=A2[:], scalar1=1.0, scalar2=None, op0=ALU.add), r=[A2], w=[A2])
            z = sb("z", [128, D])
            t8 = z
            dve.emit(lambda: nc.vector.tensor_tensor(out=t8[:], in0=l1b[:], in1=A2[:], op=ALU.mult), r=[l1b, A2], w=[t8])
            dve.emit(lambda: nc.vector.tensor_tensor(out=B2[:], in0=B2[:], in1=t8[:], op=ALU.add), r=[B2, t8], w=[B2])
            dve.emit(lambda: nc.vector.tensor_tensor(out=A2[:], in0=A2[:], in1=l1w[:], op=ALU.mult), r=[A2, l1w], w=[A2])
            rwT = sb("rwT", [128, 16, 36])
            fw.dma(sp, rwT, rwT[:], None, rw_d.rearrange("(kc p) n -> p kc n", p=128))
            rbb = sb("rbb", [128, 36])
            fw.dma(sp, rbb, rbb[:], None, rb_d.partition_broadcast(128))
            ecap = sb("ecap", [128, 32])
            fw.dma(sp, ecap, ecap[:], None, ecap_d.partition_broadcast(128))
            CNT = sb("CNT", [128, 32])
            dve.emit(lambda: nc.vector.memset(CNT[:], 0.0), w=[CNT])
            zt = sb("zt", [128, D], BF16)
            dve.emit(lambda: nc.vector.memset(zt[:], 0.0), w=[zt])
            for blk in range(NS // 128 + 1):
                fw.dma(sp, XSD, xs_d[blk * 128:(blk + 1) * 128, :], zt, zt[:], sem="st")
            fw.dma(sp, YSD, ys_d[NS:NS + 128, :], zt, zt[:], sem="st")
            XSD.w = YSD.w

            yt = [sb("yt%d" % i, [128, D], BF16) for i in range(2)]
            yT = [sb("yT%d" % i, [128, 16, 128], BF16) for i in range(2)]
            xt = [sb("xt%d" % i, [128, D]) for i in range(2)]
            ptl = [sb("ptl0", [128, D])] * 2
            x1 = [sb("x1_0", [128, D])] * 2
            h2f = sb("h2f", [128, D])
            h2b = [sb("h2b%d" % i, [128, D], BF16) for i in range(2)]
            h2T = sb("h2T", [128, 16, 128])
            bst4 = sb("bst4", [128, 4, 6])
            sm4 = [sb("sm4_%d" % i, [128, 16]) for i in range(2)]
            R = [sb("R%d" % i, [128, 256]) for i in range(2)]
            po = psm("po", [128, 4, 512])
            ptb = [psm("ptb%d" % k_, [128, 4, 128], BF16) for k_ in range(2)]
            ptf = psm("ptf", [128, 4, 128])
            pr = psm("pr", [128, 3, 64])

            def loads(i):
                a, b_, c_ = xt[i % 2], ptl[i % 2], yt[i % 2]
                fw.dma(sp, c_, c_[:], YD, y_d[i * 128:(i + 1) * 128, :])
                fw.dma(sp, a, a[:], None, x_d[i * 128:(i + 1) * 128, :])
                fw.dma(sp, b_, b_[:], None, pos_d[i * 128:(i + 1) * 128, :])

            loads(0)
            for i in range(NT):
                k2 = i % 2
                a, b_, c_, yT_ = xt[k2], ptl[k2], yt[k2], yT[k2]
                pool.emit(lambda: nc.gpsimd.tensor_tensor(out=a[:], in0=a[:], in1=b_[:], op=ALU.add), r=[a, b_], w=[a])
                if i + 1 < NT:
                    loads(i + 1)
                for g4 in range(4):
                    p_ = ptb[g4 % 2]
                    for q4 in range(4):
                        kc = g4 * 4 + q4
                        pe.emit(lambda: nc.tensor.transpose(out=p_[:, q4, :], in_=c_[:, kc * 128:(kc + 1) * 128], identity=identb[:]),
                                r=[c_, identb], w=[p_], inc=(q4 == 3))
                    (act if g4 % 2 else dve).emit(
                        (lambda: nc.scalar.copy(out=yT_[:, g4 * 4:(g4 + 1) * 4, :], in_=p_[:])) if g4 % 2 else
                        (lambda: nc.vector.tensor_copy(out=yT_[:, g4 * 4:(g4 + 1) * 4, :], in_=p_[:])), r=[p_], w=[yT_])
                for cg in range(4):
                    for kc in range(16):
                        pe.emit(lambda: nc.tensor.matmul(po[:, cg, :], lhsT=yT_[:, kc, :], rhs=wo[:, kc, cg * 512:(cg + 1) * 512],
                                                         start=(kc == 0), stop=(kc == 15)), r=[yT_, wo], w=[po], inc=(kc == 15 and cg == 3))
                for cg in range(4):
                    cs = slice(cg * 512, (cg + 1) * 512)
                    dve.emit(lambda: nc.vector.tensor_tensor(out=z[:, cs], in0=po[:, cg, :], in1=g1b[:, cs], op=ALU.mult), r=[po, g1b], w=[z])
                dve.emit(lambda: nc.vector.scalar_tensor_tensor(out=z[:], in0=a[:], scalar=ALPHA, in1=z[:], op0=ALU.mult, op1=ALU.add), r=[a, z], w=[z])
                s_ = sm4[k2]
                for cg in range(4):
                    dve.emit(lambda: nc.vector.bn_stats(out=bst4[:, cg, :], in_=z[:, cg * 512:(cg + 1) * 512]), r=[z], w=[bst4])
                dve.emit(lambda: nc.vector.bn_aggr(out=s_[:, 0:2], in_=bst4[:].rearrange("p a b -> p (a b)")), r=[bst4], w=[s_])
                dve.emit(lambda: nc.vector.tensor_scalar(out=s_[:, 2:3], in0=s_[:, 1:2], scalar1=EPS, scalar2=None, op0=ALU.add), r=[s_], w=[s_])
                act.emit(lambda: nc.scalar.activation(out=s_[:, 3:4], in_=s_[:, 2:3], func=AF.Sqrt), r=[s_], w=[s_])
                dve.emit(lambda: nc.vector.reciprocal(out=s_[:, 4:5], in_=s_[:, 3:4]), r=[s_], w=[s_])
                dve.emit(lambda: nc.vector.scalar_tensor_tensor(out=s_[:, 5:6], in0=s_[:, 0:1], scalar=-1.0, in1=s_[:, 4:5], op0=ALU.mult, op1=ALU.mult), r=[s_], w=[s_])
                act.emit(lambda: nc.scalar.activation(out=z[:], in_=z[:], func=AF.Identity, scale=s_[:, 4:5], bias=s_[:, 5:6]), r=[z, s_], w=[z])
                x1_ = x1[k2]
                pool.emit(lambda: nc.gpsimd.tensor_tensor(out=x1_[:], in0=z[:], in1=l1w[:], op=ALU.mult), r=[z, l1w], w=[x1_])
                pool.emit(lambda: nc.gpsimd.tensor_tensor(out=x1_[:], in0=x1_[:], in1=l1b[:], op=ALU.add), r=[x1_, l1b], w=[x1_])
                fw.dma(sp, X1D, x1_d[i * 128:(i + 1) * 128, :], x1_, x1_[:], sem="st")
                dve.emit(lambda: nc.vector.tensor_tensor(out=h2f[:], in0=z[:], in1=A2[:], op=ALU.mult), r=[z, A2], w=[h2f])
                dve.emit(lambda: nc.vector.tensor_tensor(out=h2f[:], in0=h2f[:], in1=B2[:], op=ALU.add), r=[h2f, B2], w=[h2f])
                hb = h2b[k2]
                act.emit(lambda: nc.scalar.copy(out=hb[:], in_=h2f[:]), r=[h2f], w=[hb])
                for g4 in range(4):
                    for q4 in range(4):
                        kc = g4 * 4 + q4
                        pe.emit(lambda: nc.tensor.transpose(out=ptf[:, q4, :], in_=h2f[:, kc * 128:(kc + 1) * 128], identity=ident[:]),
                                r=[h2f, ident], w=[ptf], inc=(q4 == 3))
                    act.emit(lambda: nc.scalar.copy(out=h2T[:, g4 * 4:(g4 + 1) * 4, :], in_=ptf[:]), r=[ptf], w=[h2T])
                for kc in range(16):
                    pe.emit(lambda: nc.tensor.matmul(pr[:, 0, 0:36], lhsT=h2T[:, kc, :], rhs=rwT[:, kc, :], start=(kc == 0), stop=(kc == 15)),
                            r=[h2T, rwT], w=[pr], inc=(kc == 15))
                r_ = R[k2]
                L = r_[:, 0:36]
                vts = lambda o_, i0, s1, op0, s2=None, op1=None: dve.emit(
                    lambda: nc.vector.tensor_scalar(out=o_, in0=i0, scalar1=s1, scalar2=s2, op0=op0, **({"op1": op1} if op1 is not None else {})), r=[r_], w=[r_])
                vtt = lambda o_, i0, i1, op: dve.emit(lambda: nc.vector.tensor_tensor(out=o_, in0=i0, in1=i1, op=op), r=[r_], w=[r_])
                vstt = lambda o_, i0, s, i1, op0, op1: dve.emit(
                    lambda: nc.vector.scalar_tensor_tensor(out=o_, in0=i0, scalar=s, in1=i1, op0=op0, op1=op1), r=[r_], w=[r_])
                vmax = lambda o_, i_: dve.emit(lambda: nc.vector.reduce_max(out=o_, in_=i_, axis=AX.X), r=[r_], w=[r_])
                vsum = lambda o_, i_: dve.emit(lambda: nc.vector.reduce_sum(out=o_, in_=i_, axis=AX.X), r=[r_], w=[r_])
                dve.emit(lambda: nc.vector.tensor_tensor(out=L, in0=pr[:, 0, 0:36], in1=rbb[:], op=ALU.add), r=[pr, rbb], w=[r_])
                c = lambda k, n=1: r_[:, k:k + n]
                m1, nm1, s1c, pgc = c(40), c(41), c(42), c(43)
                G1 = c(44, 4)
                E4 = c(48, 4)
                L2S = c(52, 8)
                ma, mb_, dd, ed, rr, g0c, g1c = c(60), c(61), c(62), c(63), c(64), c(65), c(66)
                OH1, L2M, OH2 = c(68, 8), c(76, 8), c(84, 8)
                E0, E1, Mm = c(96, 32), c(128, 32), c(160, 32)
                RK, SV, V01 = c(192, 32), c(224, 32), c(36, 4)
                vmax(m1, L[:, 0:4])
                vts(G1, L[:, 0:4], m1, ALU.is_equal)
                vts(nm1, m1, -1.0, ALU.mult)
                act.emit(lambda: nc.scalar.activation(out=E4, in_=L[:, 0:4], func=AF.Exp, bias=nm1, scale=1.0), r=[r_], w=[r_])
                vsum(s1c, E4)
                dve.emit(lambda: nc.vector.reciprocal(out=pgc, in_=s1c), r=[r_], w=[r_])
                vts(L2S, L[:, 4:12], G1[:, 0:1], ALU.mult)
                for g in range(1, 4):
                    vstt(L2S, L[:, 4 + 8 * g:12 + 8 * g], G1[:, g:g + 1], L2S, ALU.mult, ALU.add)
                vmax(ma, L2S)
                vts(OH1, L2S, ma, ALU.is_equal)
                vstt(L2M, OH1, -1e30, L2S, ALU.mult, ALU.add)
                vmax(mb_, L2M)
                vts(OH2, L2M, mb_, ALU.is_equal)
                vtt(dd, mb_, ma, ALU.subtract)
                act.emit(lambda: nc.scalar.activation(out=ed, in_=dd, func=AF.Exp), r=[r_], w=[r_])
                vts(rr, ed, 1.0, ALU.add)
                dve.emit(lambda: nc.vector.reciprocal(out=rr, in_=rr), r=[r_], w=[r_])
                vtt(g0c, pgc, rr, ALU.mult)
                vtt(g1c, g0c, ed, ALU.mult)
                dve.emit(lambda: nc.vector.tensor_copy(out=GT[:, i, 0:1], in_=g0c), r=[r_], w=[GT])
                dve.emit(lambda: nc.vector.tensor_copy(out=GT[:, i, 1:2], in_=g1c), r=[r_], w=[GT])
                for g in range(4):
                    vts(E0[:, g * 8:(g + 1) * 8], OH1, G1[:, g:g + 1], ALU.mult)
                    vts(E1[:, g * 8:(g + 1) * 8], OH2, G1[:, g:g + 1], ALU.mult)
                vtt(Mm, E0, E1, ALU.add)
                pe.emit(lambda: nc.tensor.matmul(pr[:, 1, 0:32], lhsT=triS[:], rhs=Mm, start=True, stop=True), r=[triS, r_], w=[pr], inc=False)
                pe.emit(lambda: nc.tensor.matmul(pr[:, 2, 0:32], lhsT=ones[:], rhs=Mm, start=True, stop=True), r=[ones, r_], w=[pr])
                dve.emit(lambda: nc.vector.tensor_tensor(out=RK, in0=pr[:, 1, 0:32], in1=CNT[:], op=ALU.add), r=[pr, CNT], w=[r_])
                dve.emit(lambda: nc.vector.tensor_tensor(out=CNT[:], in0=pr[:, 2, 0:32], in1=CNT[:], op=ALU.add), r=[pr, CNT], w=[CNT])
                vts(SV, RK, float(CAP), ALU.is_lt)
                dve.emit(lambda: nc.vector.tensor_tensor(out=RK, in0=RK, in1=ecap[:], op=ALU.add), r=[r_, ecap], w=[r_])
                vts(RK, RK, -float(NS), ALU.add)
                vtt(RK, RK, SV, ALU.mult)
                vts(RK, RK, float(NS), ALU.add)
                sf = c(36, 2)
                vtt(E0, E0, RK, ALU.mult)
                vtt(E1, E1, RK, ALU.mult)
                vsum(sf[:, 0:1], E0)
                vsum(sf[:, 1:2], E1)
                dve.emit(lambda: nc.vector.tensor_copy(out=SLT[:, i, :], in_=sf), r=[r_], w=[SLT])
                for k in range(2):
                    fw.dma(pool, XSD, xs_d, hb, hb[:], sem="ind", idx_t=SLT,
                           out_offset=bass.IndirectOffsetOnAxis(ap=SLT[:, i, k:k + 1], axis=0), in_offset=None)
            fw.barrier()
        if stage == 4:
            o = dout("dbg_x1", [2048, D])
            with nc.sbuf_tensor("xtmp", [128, 4, D], F32) as yt_:
                YT_ = T(yt_)
                for q in range(4):
                    fw.dma(sp, YT_, yt_[:], X1D, x1_d[q * 512:(q + 1) * 512].rearrange("(a p) d -> p a d", p=128))
                    fw.dma(sp, None, o[q * 512:(q + 1) * 512].rearrange("(a p) d -> p a d", p=128), YT_, yt_[:], sem="st")
            o = dout("dbg_slt", [128, NT * 2], I32)
            fw.dma(sp, None, o, SLT, SLT[:].rearrange("p a b -> p (a b)"), sem="st")
            o = dout("dbg_gt", [128, NT * 2])
            fw.dma(sp, None, o, GT, GT[:].rearrange("p a b -> p (a b)"), sem="st")
        if stage == 4:
            fw.barrier()
            return nc, names, dbg_out

        with contextlib.ExitStack() as ph:
            sb, psm = mk(ph, "sb"), mk(ph, "ps")
            wp = [sb("wp%d" % i, [128, 16384], BF16) for i in range(4)]
            wp_n = [0]
            xs = [sb("xs%d" % i, [128, 4, D], BF16) for i in range(1)]
            xT = sb("xT", [128, 16, CAP], BF16)
            hT = sb("hT", [128, 8, CAP], BF16)
            sg = [sb("sg%d" % i, [128, CAP]) for i in range(2)]
            ye = [sb("ye%d" % i, [128, D], BF16) for i in range(2)]
            ptb = [psm("ptb%d" % k_, [128, 4, 128], BF16) for k_ in range(2)]
            pgu = [psm("pgu%d" % i, [128, CAP]) for i in range(2)]
            py = psm("py", [128, 4, 512])
            n_y = 0
            for e in range(NE):
                slots = []
                for (src, kcn, ncol) in ((wg_d[e], 16, 1024), (wu_d[e], 16, 1024), (wd_d[e], 8, 2048)):
                    s_ = wp[wp_n[0] % 4]
                    wp_n[0] += 1
                    v_ = s_[:].rearrange("p (k n) -> p k n", k=kcn)
                    sv = src.rearrange("(kc p) n -> p kc n", p=128)
                    hk = kcn // 2
                    for hh in range(2):
                        fw.dma(pool, s_, v_[:, hh * hk:(hh + 1) * hk, :], None, sv[:, hh * hk:(hh + 1) * hk, :], sem="wld")
                    slots.append((s_, v_))
                (Wg, Wgv), (Wu, Wuv), (Wd, Wdv) = slots
                xs_ = xs[0]
                fw.dma(sp, xs_, xs_[:], XSD, xs_d[e * CAP:(e + 1) * CAP, :].rearrange("(b p) d -> p b d", p=128))
                nt_ = 0
                for b in range(4):
                    for g4 in range(4):
                        p_ = ptb[nt_ % 2]
                        nt_ += 1
                        for q4 in range(4):
                            kc = g4 * 4 + q4
                            pe.emit(lambda: nc.tensor.transpose(out=p_[:, q4, :], in_=xs_[:, b, kc * 128:(kc + 1) * 128], identity=identb[:]),
                                    r=[xs_, identb], w=[p_], inc=(q4 == 3))
                        (act if nt_ % 2 else dve).emit(
                            (lambda: nc.scalar.copy(out=xT[:, g4 * 4:(g4 + 1) * 4, b * 128:(b + 1) * 128], in_=p_[:])) if nt_ % 2 else
                            (lambda: nc.vector.tensor_copy(out=xT[:, g4 * 4:(g4 + 1) * 4, b * 128:(b + 1) * 128], in_=p_[:])), r=[p_], w=[xT])
                for j in range(8):
                    pg_, pu_ = pgu[0], pgu[1]
                    for kc in range(16):
                        pe.emit(lambda: nc.tensor.matmul(pg_[:], lhsT=Wgv[:, kc, j * 128:(j + 1) * 128], rhs=xT[:, kc, :], start=(kc == 0), stop=(kc == 15)),
                                r=[Wg, xT], w=[pg_], inc=(kc == 15))
                    for kc in range(16):
                        pe.emit(lambda: nc.tensor.matmul(pu_[:], lhsT=Wuv[:, kc, j * 128:(j + 1) * 128], rhs=xT[:, kc, :], start=(kc == 0), stop=(kc == 15)),
                                r=[Wu, xT], w=[pu_], inc=(kc == 15))
                    s_ = sg[j % 2]
                    act.emit(lambda: nc.scalar.activation(out=s_[:], in_=pg_[:], func=AF.Silu), r=[pg_], w=[s_])
                    dve.emit(lambda: nc.vector.tensor_tensor(out=hT[:, j, :], in0=s_[:], in1=pu_[:], op=ALU.mult), r=[s_, pu_], w=[hT])
                for b in range(4):
                    for cg in range(4):
                        for kc in range(8):
                            pe.emit(lambda: nc.tensor.matmul(py[:, cg, :], lhsT=hT[:, kc, b * 128:(b + 1) * 128], rhs=Wdv[:, kc, cg * 512:(cg + 1) * 512],
                                                             start=(kc == 0), stop=(kc == 7)), r=[hT, Wd], w=[py], inc=(kc == 7 and cg == 3))
                    y_ = ye[n_y % 2]
                    n_y += 1
                    for cg in range(4):
                        cs = slice(cg * 512, (cg + 1) * 512)
                        if cg % 2:
                            act.emit(lambda: nc.scalar.copy(out=y_[:, cs], in_=py[:, cg, :]), r=[py], w=[y_])
                        else:
                            dve.emit(lambda: nc.vector.tensor_copy(out=y_[:, cs], in_=py[:, cg, :]), r=[py], w=[y_])
                    fw.dma(sp, YSD, ys_d[e * CAP + b * 128:e * CAP + (b + 1) * 128, :], y_, y_[:], sem="st")
            fw.barrier()

        with contextlib.ExitStack() as ph:
            sb, psm = mk(ph, "sb"), mk(ph, "ps")
            g2b = sb("g2b", [128, D])
            l2w = sb("l2w", [128, D])
            l2b = sb("l2b", [128, D])
            fw.dma(sp, g2b, g2b[:], MOD, mod_d[0, 5 * D:6 * D].partition_broadcast(128))
            fw.dma(sp, l2w, l2w[:], None, ln2w_d.partition_broadcast(128))
            fw.dma(sp, l2b, l2b[:], None, ln2b_d.partition_broadcast(128))
            y0 = [sb("y0_%d" % i, [128, D], BF16) for i in range(2)]
            y1 = [sb("y1_%d" % i, [128, D], BF16) for i in range(2)]
            x1 = [sb("x1_%d" % i, [128, D]) for i in range(2)]
            mo = sb("mo", [128, D])
            ot = [sb("ot%d" % i, [128, D]) for i in range(2)]
            bst4 = sb("bst4", [128, 4, 6])
            sm4 = [sb("sm4_%d" % i, [128, 16]) for i in range(2)]

            def loads6(i):
                k2 = i % 2
                fw.dma(pool, y0[k2], y0[k2][:], YSD, ys_d, sem="ind", idx_t=SLT, out_offset=None,
                       in_offset=bass.IndirectOffsetOnAxis(ap=SLT[:, i, 0:1], axis=0))
                fw.dma(pool, y1[k2], y1[k2][:], YSD, ys_d, sem="ind", idx_t=SLT, out_offset=None,
                       in_offset=bass.IndirectOffsetOnAxis(ap=SLT[:, i, 1:2], axis=0))
                fw.dma(sp, x1[k2], x1[k2][:], X1D, x1_d[i * 128:(i + 1) * 128, :])

            loads6(0)
            for i in range(NT):
                if i + 1 < NT:
                    loads6(i + 1)
                k2 = i % 2
                a0, a1, xx, o_ = y0[k2], y1[k2], x1[k2], ot[k2]
                dve.emit(lambda: nc.vector.tensor_scalar(out=mo[:], in0=a0[:], scalar1=GT[:, i, 0:1], scalar2=None, op0=ALU.mult), r=[a0, GT], w=[mo])
                dve.emit(lambda: nc.vector.scalar_tensor_tensor(out=mo[:], in0=a1[:], scalar=GT[:, i, 1:2], in1=mo[:], op0=ALU.mult, op1=ALU.add),
                         r=[a1, GT, mo], w=[mo])
                dve.emit(lambda: nc.vector.tensor_tensor(out=mo[:], in0=mo[:], in1=g2b[:], op=ALU.mult), r=[mo, g2b], w=[mo])
                dve.emit(lambda: nc.vector.scalar_tensor_tensor(out=mo[:], in0=xx[:], scalar=ALPHA, in1=mo[:], op0=ALU.mult, op1=ALU.add), r=[xx, mo], w=[mo])
                s_ = sm4[k2]
                for cg in range(4):
                    dve.emit(lambda: nc.vector.bn_stats(out=bst4[:, cg, :], in_=mo[:, cg * 512:(cg + 1) * 512]), r=[mo], w=[bst4])
                dve.emit(lambda: nc.vector.bn_aggr(out=s_[:, 0:2], in_=bst4[:].rearrange("p a b -> p (a b)")), r=[bst4], w=[s_])
                dve.emit(lambda: nc.vector.tensor_scalar(out=s_[:, 2:3], in0=s_[:, 1:2], scalar1=EPS, scalar2=None, op0=ALU.add), r=[s_], w=[s_])
                act.emit(lambda: nc.scalar.activation(out=s_[:, 3:4], in_=s_[:, 2:3], func=AF.Sqrt), r=[s_], w=[s_])
                dve.emit(lambda: nc.vector.reciprocal(out=s_[:, 4:5], in_=s_[:, 3:4]), r=[s_], w=[s_])
                dve.emit(lambda: nc.vector.scalar_tensor_tensor(out=s_[:, 5:6], in0=s_[:, 0:1], scalar=-1.0, in1=s_[:, 4:5], op0=ALU.mult, op1=ALU.mult), r=[s_], w=[s_])
                act.emit(lambda: nc.scalar.activation(out=mo[:], in_=mo[:], func=AF.Identity, scale=s_[:, 4:5], bias=s_[:, 5:6]), r=[mo, s_], w=[mo])
                dve.emit(lambda: nc.vector.tensor_tensor(out=o_[:], in0=mo[:], in1=l2w[:], op=ALU.mult), r=[mo, l2w], w=[o_])
                dve.emit(lambda: nc.vector.tensor_tensor(out=o_[:], in0=o_[:], in1=l2b[:], op=ALU.add), r=[o_, l2b], w=[o_])
                fw.dma(sp, None, out_d[i * 128:(i + 1) * 128, :], o_, o_[:], sem="out")
            fw.barrier()
    return nc, names, dbg_out


def _pos_table():
    rows, gw, q = 32, 64, 512
    r = np.repeat(np.arange(rows, dtype=np.float32), gw)
    col = np.tile(np.arange(gw, dtype=np.float32), rows)
    omega = (1.0 / (np.float32(10000.0) ** (np.arange(q, dtype=np.float32) / np.float32(q)))).astype(np.float32)
    ar = r[:, None] * omega
    ac = col[:, None] * omega
    return np.concatenate([np.sin(ar), np.cos(ar), np.sin(ac), np.cos(ac)], -1).astype(np.float32)


def _consts():
    t = np.arange(128)
    c = {}
    c["ident"] = np.eye(128, dtype=np.float32)
    c["triF"] = (t[:, None] <= t[None, :]).astype(np.float32)
    c["triB"] = (t[:, None] >= t[None, :]).astype(np.float32)
    c["triS"] = (t[:, None] < t[None, :]).astype(np.float32)
    c["maskF"] = np.where(t[:, None] <= t[None, :], 0.0, NEG).astype(np.float32)
    c["maskB"] = np.where(t[:, None] >= t[None, :], 0.0, NEG).astype(np.float32)
    c["ecap"] = (np.arange(32) * CAP).astype(np.float32)
    c["pos"] = _pos_table()
    return c


def make_in_map(inp, b, names, consts):
    f = lambda a: np.ascontiguousarray(a, dtype=np.float32)
    m = {}
    m["x"] = f(inp["x"][b])
    m["ctx"] = f(inp["ctx"][b])
    cc = np.stack([inp["c"][b], inp["c_ctx"]], -1)
    m["ccT"] = f(cc.reshape(16, 128, 2).transpose(1, 0, 2))
    m["w_mod"] = f(inp["w_mod"][0])
    m["b_mod"] = f(inp["b_mod"][0])
    m["w_in"] = f(inp["w_in"][0])
    m["conv_wT"] = f(inp["conv_w"][0].reshape(3, 16, 128).transpose(2, 0, 1))
    m["conv_bT"] = f(inp["conv_b"][0].reshape(16, 128).T)
    m["gate_bias"] = f(inp["gate_bias"][0].reshape(16))
    m["mlstm_norm_w"] = f(inp["mlstm_norm_w"][0])
    m["cmlp_norm_w"] = f(inp["cmlp_norm_w"][0])
    m["w_sT"] = f(inp["w_s"][0].transpose(0, 2, 1))
    m["b_sT"] = f(inp["b_s"][0].T)
    if "w_out" in names:
        m["w_out"] = f(inp["w_out"][0])
        m["ln1_w"] = f(inp["ln1_w"][0])
        m["ln1_b"] = f(inp["ln1_b"][0])
        m["rw"] = f(np.concatenate([inp["router1_w"][0], inp["router2_w"][0]], -1))
        m["rb"] = f(np.concatenate([inp["router1_b"][0], inp["router2_b"][0]], -1))
    if "w_gate" in names:
        m["w_gate"] = f(inp["w_gate"][0])
        m["w_up"] = f(inp["w_up"][0])
        m["w_down"] = f(inp["w_down"][0])
        m["ln2_w"] = f(inp["ln2_w"][0])
        m["ln2_b"] = f(inp["ln2_b"][0])
    for k, v in consts.items():
        if k in names:
            m[k] = v
    return {k: v for k, v in m.items() if k in names}


def kernel(**inputs):
    inp = {k: np.asarray(v) for k, v in inputs.items()}
    nc, names, _ = build()
    consts = _consts()
    in_maps = [make_in_map(inp, b, names, consts) for b in range(8)]
    res = run_bass_kernel_spmd(nc, in_maps, core_ids=list(range(8)))
    return np.stack([np.asarray(r["out"], dtype=np.float32) for r in res.results], 0)
```

```python
import os
import contextlib
import numpy as np
import concourse.bass as bass
import concourse.mybir as mybir
from concourse.bass_utils import run_bass_kernel_spmd

F32 = mybir.dt.float32
BF16 = mybir.dt.bfloat16
I32 = mybir.dt.int32
AF = mybir.ActivationFunctionType
ALU = mybir.AluOpType
AX = mybir.AxisListType

D = 2048
NT = 16
NTT = 18
NTOK = 2304
CAP = 512
NE = 32
NS = NE * CAP
ALPHA = 2.0 ** 0.25
EPS = 1e-6
NEG = -30000.0


class Tok:
    __slots__ = ("sem", "val", "eng")

    def __init__(self, sem, val, eng=None):
        self.sem = sem
        self.val = val
        self.eng = eng


class T:
    def __init__(self, t, name="", dram=False):
        self.t = t
        self.w = None
        self.r = []
        self.name = name
        self.dram = dram

    def __getitem__(self, idx):
        return self.t[idx]


class Eng:
    def __init__(self, name, h, sem, same_wait=True):
        self.name = name
        self.h = h
        self.sem = sem
        self.count = 0
        self.waited = {}
        self.pending = []
        self.last_ins = None
        self.last_has_inc = True
        self.same_wait = same_wait

    def flush(self):
        if not self.pending:
            return
        if not self.last_has_inc:
            self.count += 1
            self.last_ins.then_inc(self.sem, 1)
            self.last_has_inc = True
        for tk in self.pending:
            tk.val = self.count
        self.pending = []

    def wait(self, tok):
        if tok is None:
            return
        if tok.val is None:
            tok.eng.flush()
        if tok.sem is self.sem and not self.same_wait:
            return
        key = id(tok.sem)
        if self.waited.get(key, 0) >= tok.val:
            return
        self.h.wait_ge(tok.sem, tok.val)
        self.waited[key] = tok.val

    def emit(self, build, r=(), w=(), inc=True):
        for t in r:
            self.wait(t.w)
        for t in w:
            self.wait(t.w)
            for rr in t.r:
                self.wait(rr)
        ins = build()
        self.last_ins = ins
        if inc:
            self.count += 1
            ins.then_inc(self.sem, 1)
            self.last_has_inc = True
            tok = Tok(self.sem, self.count, self)
            for tk in self.pending:
                tk.val = self.count
            self.pending = []
        else:
            self.last_has_inc = False
            tok = Tok(self.sem, None, self)
            self.pending.append(tok)
        for t in r:
            t.r.append(tok)
        for t in w:
            t.w = tok
            t.r = []
        return tok


class FW:
    def __init__(self, nc, stack):
        self.nc = nc
        self.stack = stack
        mk = lambda n: stack.enter_context(nc.semaphore(n))
        self.pe = Eng("pe", nc.tensor, mk("s_pe"), same_wait=False)
        sw = os.environ.get("KSAME", "1") == "1"
        self.act = Eng("act", nc.scalar, mk("s_act"), same_wait=sw)
        self.dve = Eng("dve", nc.vector, mk("s_dve"), same_wait=sw)
        self.pool = Eng("pool", nc.gpsimd, mk("s_pool"))
        self.sp = Eng("sp", nc.sync, mk("s_sp"))
        self.engs = [self.pe, self.act, self.dve, self.pool, self.sp]
        self.dsems = {}
        self.dcount = {}

    def dsem(self, name):
        if name not in self.dsems:
            self.dsems[name] = self.stack.enter_context(self.nc.semaphore("d_" + name))
            self.dcount[name] = 0
        return self.dsems[name]

    def dma(self, q, out_t, out_ap, in_t, in_ap, sem=None, idx_t=None, **kw):
        sbt = out_t if (out_t is not None and not out_t.dram) else in_t
        if sem is None or not sem.startswith("grp_"):
            sem = "t_" + sbt.name
        s = self.dsem(sem)
        if in_t is not None:
            q.wait(in_t.w)
        if out_t is not None:
            q.wait(out_t.w)
            for rr in out_t.r:
                q.wait(rr)
        if idx_t is not None:
            q.wait(idx_t.w)
            ins = q.h.indirect_dma_start(out=out_ap, in_=in_ap, **kw)
        else:
            ins = q.h.dma_start(out=out_ap, in_=in_ap, **kw)
        self.dcount[sem] += 16
        ins.then_inc(s, 16)
        tok = Tok(s, self.dcount[sem])
        if in_t is not None:
            in_t.r.append(tok)
        if idx_t is not None:
            idx_t.r.append(tok)
        if out_t is not None:
            out_t.w = tok
            out_t.r = []
        return tok

    def group_done(self, sem, tiles):
        tok = Tok(self.dsems[sem], self.dcount[sem])
        for t in tiles:
            t.w = tok

    def barrier(self):
        for e in self.engs:
            e.flush()
        toks = [Tok(e.sem, e.count) for e in self.engs if e.count > 0]
        toks += [Tok(self.dsems[n], self.dcount[n]) for n in self.dsems if self.dcount[n] > 0]
        for e in self.engs:
            for tk in toks:
                if tk.sem is e.sem and not e.same_wait:
                    continue
                e.wait(tk)


def build(stage=99, dbg=None):
    nc = bass.Bass("TRN2", target_bir_lowering=False)
    names = {}

    def din(name, shape, dt=F32):
        names[name] = nc.dram_tensor(name, list(shape), dt, kind="ExternalInput").ap()
        return names[name]

    x_d = din("x", [2048, D])
    ctx_d = din("ctx", [256, D])
    pos_d = din("pos", [2048, D])
    ccT_d = din("ccT", [128, 16, 2])
    wmod_d = din("w_mod", [D, 6 * D])
    bmod_d = din("b_mod", [6 * D])
    win_d = din("w_in", [D, 6160])
    cwT_d = din("conv_wT", [128, 3, 16])
    cbT_d = din("conv_bT", [128, 16])
    gb_d = din("gate_bias", [16])
    mnw_d = din("mlstm_norm_w", [1024])
    cnw_d = din("cmlp_norm_w", [1024])
    wsT_d = din("w_sT", [4, 128, 128])
    bsT_d = din("b_sT", [128, 4])
    ident_d = din("ident", [128, 128])
    triF_d = din("triF", [128, 128])
    triB_d = din("triB", [128, 128])
    triS_d = din("triS", [128, 128])
    mF_d = din("maskF", [128, 128])
    mB_d = din("maskB", [128, 128])
    if stage >= 4:
        wout_d = din("w_out", [D, D])
        ln1w_d = din("ln1_w", [D])
        ln1b_d = din("ln1_b", [D])
        rw_d = din("rw", [D, 36])
        rb_d = din("rb", [36])
        ecap_d = din("ecap", [32])
    if stage >= 5:
        wg_d = din("w_gate", [NE, D, 1024])
        wu_d = din("w_up", [NE, D, 1024])
        wd_d = din("w_down", [NE, 1024, D])
        ln2w_d = din("ln2_w", [D])
        ln2b_d = din("ln2_b", [D])
        out_d = nc.dram_tensor("out", [2048, D], F32, kind="ExternalOutput").ap()

    mod_d = nc.dram_tensor("mod_scr", [2, 6 * D], F32, kind="Internal").ap()
    y_d = nc.dram_tensor("y_scr", [2048, D], BF16, kind="Internal").ap()
    x1_d = nc.dram_tensor("x1_scr", [2048, D], F32, kind="Internal").ap()
    xs_d = nc.dram_tensor("xs_scr", [NS + 128, D], BF16, kind="Internal").ap()
    ys_d = nc.dram_tensor("ys_scr", [NS + 128, D], BF16, kind="Internal").ap()
    MOD, YD, X1D, XSD, YSD = [T(a_, n_, dram=True) for a_, n_ in ((mod_d, "MOD"), (y_d, "YD"), (x1_d, "X1D"), (xs_d, "XSD"), (ys_d, "YSD"))]
    dbg_out = {}

    def dout(name, shape, dt=F32):
        dbg_out[name] = nc.dram_tensor(name, list(shape), dt, kind="ExternalOutput").ap()
        return dbg_out[name]

    with contextlib.ExitStack() as top:
        fw = FW(nc, top)
        pe, act, dve, pool, sp = fw.pe, fw.act, fw.dve, fw.pool, fw.sp

        uid = [0]

        def mk(st, kind):
            def f(name, shape, dt=F32):
                a = nc.sbuf_tensor if kind == "sb" else nc.psum_tensor
                uid[0] += 1
                return T(st.enter_context(a("%s_%s%d" % (kind, name, uid[0]), list(shape), dt)), name)
            return f

        sbG = mk(top, "sb")
        ident = sbG("ident", [128, 128])
        identb = sbG("identb", [128, 128], BF16)
        triF = sbG("triF", [128, 128])
        triB = sbG("triB", [128, 128])
        triS = sbG("triS", [128, 128])
        mF = sbG("mF", [128, 128])
        mB = sbG("mB", [128, 128])
        ones = sbG("ones", [128, 128])
        for t_, d_ in ((ident, ident_d), (triF, triF_d), (triB, triB_d), (triS, triS_d), (mF, mF_d), (mB, mB_d)):
            fw.dma(sp, t_, t_[:], None, d_, sem="grp_c0")
        fw.group_done("grp_c0", [ident, triF, triB, triS, mF, mB])
        dve.emit(lambda: nc.vector.tensor_copy(out=identb[:], in_=ident[:]), r=[ident], w=[identb])
        dve.emit(lambda: nc.vector.memset(ones[:], 1.0), w=[ones])
        zt = sbG("zt", [128, 1024], BF16)
        sc1 = sbG("sc1", [128, 4, 16])

        with contextlib.ExitStack() as ph:
            sb, psm = mk(ph, "sb"), mk(ph, "ps")
            cc = sb("cc", [128, 16, 2])
            sil = sb("sil", [128, 16, 2], BF16)
            bm = sb("bm", [2, 6 * D])
            modsb = sb("modsb", [2, 6 * D])
            wm = [sb("wm%d" % i, [128, 16, 1536], BF16) for i in range(2)]
            pm = [psm("pm%d" % i, [2, 3, 512]) for i in range(2)]
            fw.dma(sp, cc, cc[:], None, ccT_d)
            fw.dma(sp, bm, bm[:], None, bmod_d.partition_broadcast(2))
            act.emit(lambda: nc.scalar.activation(out=sil[:], in_=cc[:], func=AF.Silu), r=[cc], w=[sil])
            wv = wmod_d.rearrange("(kc p) n -> p kc n", p=128)
            for cg in range(8):
                w_ = wm[cg % 2]
                for hh in range(2):
                    fw.dma(pool, w_, w_[:, hh * 8:(hh + 1) * 8, :], None,
                           wv[:, hh * 8:(hh + 1) * 8, cg * 1536:(cg + 1) * 1536], sem="wld")
                p_ = pm[cg % 2]
                for j in range(3):
                    for kc in range(16):
                        pe.emit(lambda: nc.tensor.matmul(p_[0:2, j, :], lhsT=sil[:, kc, :], rhs=w_[:, kc, j * 512:(j + 1) * 512],
                                                         start=(kc == 0), stop=(kc == 15)),
                                r=[sil, w_], w=[p_], inc=(j == 2 and kc == 15))
                dve.emit(lambda: nc.vector.tensor_tensor(out=modsb[0:2, cg * 1536:(cg + 1) * 1536],
                                                         in0=p_[0:2, :, :].rearrange("p a b -> p (a b)"),
                                                         in1=bm[0:2, cg * 1536:(cg + 1) * 1536], op=ALU.add),
                         r=[p_, bm], w=[modsb])
            fw.dma(sp, MOD, mod_d, modsb, modsb[0:2, :], sem="st")
            t16 = sb("t16", [16, 4, 128])
            pt16 = psm("pt16", [128, 4, 16])
            for v, (row, n) in enumerate(((0, 0), (0, 1), (1, 0), (1, 1))):
                fw.dma(sp, t16, t16[0:16, v, :], MOD, mod_d[row, n * D:(n + 1) * D].rearrange("(kc p) -> kc p", p=128))
            for v in range(4):
                pe.emit(lambda: nc.tensor.transpose(out=pt16[:, v, :], in_=t16[0:16, v, :], identity=ident[0:16, 0:16]),
                        r=[t16, ident], w=[pt16], inc=(v == 3))
            dve.emit(lambda: nc.vector.tensor_copy(out=sc1[:], in_=pt16[:]), r=[pt16], w=[sc1])
            for v in (1, 3):
                dve.emit(lambda: nc.vector.tensor_scalar(out=sc1[:, v, :], in0=sc1[:, v, :], scalar1=1.0, scalar2=None, op0=ALU.add),
                         r=[sc1], w=[sc1])
            if stage == 1:
                o = dout("dbg_mod", [2, 6 * D])
                fw.dma(sp, None, o, modsb, modsb[0:2, :], sem="st")
                o2 = dout("dbg_sc1", [128, 64])
                fw.dma(sp, None, o2, sc1, sc1[:].rearrange("p a b -> p (a b)"), sem="st")
            fw.barrier()
        if stage == 1:
            return nc, names, dbg_out

        with contextlib.ExitStack() as ph:
            sb, psm = mk(ph, "sb"), mk(ph, "ps")
            hxT = sb("hxT", [128, 16, NTOK], BF16)
            zf_jobs = []
            if stage >= 4:
                dve.emit(lambda: nc.vector.memset(zt[:], 0.0), w=[zt])
                zsrc = zt[:].unsqueeze(1).to_broadcast([128, 32, 1024])
                zsrc2 = zt[:].unsqueeze(1).to_broadcast([128, 2, 1024])
                for blk in range(NS // 2048):
                    zf_jobs.append((XSD, xs_d[blk * 2048:(blk + 1) * 2048, :].rearrange("(p a) d -> p (a d)", a=16).rearrange("p (c d) -> p c d", d=1024), zsrc))
                zf_jobs.append((XSD, xs_d[NS:NS + 128, :].rearrange("p (h d) -> p h d", h=2), zsrc2))
                zf_jobs.append((YSD, ys_d[NS:NS + 128, :].rearrange("p (h d) -> p h d", h=2), zsrc2))

            def zf_issue(n):
                for _ in range(n):
                    if zf_jobs:
                        t_, dst, src = zf_jobs.pop(0)
                        fw.dma(sp, t_, dst, zt, src)
                        XSD.w = t_.w
                        YSD.w = t_.w
            with contextlib.ExitStack() as p2:
                sb2, ps2 = mk(p2, "sb"), mk(p2, "ps")
                xt = [sb2("xt%d" % i, [128, D]) for i in range(2)]
                ptl = [sb2("ptl%d" % i, [128, D]) for i in range(2)]
                ptr = [ps2("ptr%d" % i, [128, 4, 128]) for i in range(3)]
                n_tr = 0
                for i in range(NTT):
                    a, b_ = xt[i % 2], ptl[i % 2]
                    if i < NT:
                        fw.dma(sp, a, a[:], None, x_d[i * 128:(i + 1) * 128, :])
                        fw.dma(sp, b_, b_[:], None, pos_d[i * 128:(i + 1) * 128, :])
                        dve.emit(lambda: nc.vector.tensor_tensor(out=a[:], in0=a[:], in1=b_[:], op=ALU.add), r=[b_, a], w=[a])
                        sv = 0
                    else:
                        fw.dma(sp, a, a[:], None, ctx_d[(i - NT) * 128:(i - NT + 1) * 128, :])
                        sv = 2
                    tk0 = NT * 128 + (i - NT) * 128 if i >= NT else i * 128
                    for g4 in range(4):
                        p_ = ptr[n_tr % 3]
                        n_tr += 1
                        for q4 in range(4):
                            kc = g4 * 4 + q4
                            pe.emit(lambda: nc.tensor.transpose(out=p_[:, q4, :], in_=a[:, kc * 128:(kc + 1) * 128], identity=ident[:]),
                                    r=[a, ident], w=[p_], inc=(q4 == 3))
                        for q4 in range(4):
                            kc = g4 * 4 + q4
                            if q4 % 2 == 0:
                                dve.emit(lambda: nc.vector.tensor_scalar(out=hxT[:, kc, tk0:tk0 + 128], in0=p_[:, q4, :],
                                                                         scalar1=sc1[:, sv + 1, kc:kc + 1], scalar2=sc1[:, sv, kc:kc + 1],
                                                                         op0=ALU.mult, op1=ALU.add), r=[p_, sc1], w=[hxT])
                            else:
                                act.emit(lambda: nc.scalar.activation(out=hxT[:, kc, tk0:tk0 + 128], in_=p_[:, q4, :], func=AF.Identity,
                                                                      scale=sc1[:, sv + 1, kc:kc + 1], bias=sc1[:, sv, kc:kc + 1]),
                                         r=[p_, sc1], w=[hxT])
                fw.barrier()
            if stage == 2:
                o = dout("dbg_hxT", [128, 16 * NTOK], BF16)
                fw.dma(sp, None, o, hxT, hxT[:].rearrange("p a b -> p (a b)"), sem="st")
                fw.barrier()
                return nc, names, dbg_out

            wsl = [sb("wsl%d" % i, [128, 16, 256], BF16) for i in range(4)]
            wsl_n = [0]
            winv = win_d.rearrange("(kc p) n -> p kc n", p=128)

            def load_w(c0, ncol=256):
                s_ = wsl[wsl_n[0] % 4]
                wsl_n[0] += 1
                fw.dma(pool, s_, s_[:, :, 0:ncol], None, winv[:, :, c0:c0 + ncol], sem="wld")
                return s_

            G = sb("G", [128, NTT, 16])
            CS = sb("CS", [128, NTT, 3, 16])
            CB8 = sb("CB8", [128, NTT, 8])
            EB8 = sb("EB8", [128, NTT, 8])
            TOT8 = sb("TOT8", [128, NTT, 8])
            W8 = sb("W8", [128, NTT, 8])
            EL8 = sb("EL8", [128, NTT, 8])
            gbc = sb("gbc", [128, 16])
            cw = sb("cw", [128, 3, 16])
            cb = sb("cb", [128, 16])
            mnw = sb("mnw", [128, 1024])
            cnw = sb("cnw", [128, 1024])
            bsT = sb("bsT", [128, 4])
            fw.dma(sp, gbc, gbc[:], None, gb_d.partition_broadcast(128), sem="grp_c3")
            fw.dma(sp, cw, cw[:], None, cwT_d, sem="grp_c3")
            fw.dma(sp, cb, cb[:], None, cbT_d, sem="grp_c3")
            fw.dma(sp, mnw, mnw[:], None, mnw_d.partition_broadcast(128), sem="grp_c3")
            fw.dma(sp, cnw, cnw[:], None, cnw_d.partition_broadcast(128), sem="grp_c3")
            fw.dma(sp, bsT, bsT[:], None, bsT_d, sem="grp_c3")
            fw.group_done("grp_c3", [gbc, cw, cb, mnw, cnw, bsT])

            pbig_t = [ph.enter_context(nc.psum_tensor("ps_pbig%d" % i, [128, 512], F32)) for i in range(2)]
            pbig = [T(t_) for t_ in pbig_t]
            pg = [T(t_[:, 0:48].rearrange("p (a b) -> p a b", a=3)) for t_ in pbig_t]
            for k_ in range(2):
                pg[k_] = pbig[k_]
            pgv = [t_[:, 0:48].rearrange("p (a b) -> p a b", a=3) for t_ in pbig_t]
            wg_ = load_w(4096, 16)
            for i in range(NTT):
                p_ = pg[i % 2]
                pv_ = pgv[i % 2]
                for kc in range(16):
                    pe.emit(lambda: nc.tensor.matmul(pv_[:, 0, :], lhsT=hxT[:, kc, i * 128:(i + 1) * 128], rhs=wg_[:, kc, 0:16],
                                                     start=(kc == 0), stop=(kc == 15)), r=[hxT, wg_], w=[p_], inc=(kc == 15))
                dve.emit(lambda: nc.vector.tensor_tensor(out=G[:, i, :], in0=pv_[:, 0, :], in1=gbc[:], op=ALU.add), r=[p_, gbc], w=[G])
            def cutG(nm):
                if stage == 3 and dbg == nm:
                    o = dout("dbg_G", [128, NTT * 16])
                    fw.dma(sp, None, o, G, G[:].rearrange("p a b -> p (a b)"), sem="st")
                    fw.barrier()
                    return True
                return False
            def cut(nm, t_, ap_, shape, dt=F32):
                if stage == 3 and dbg == nm:
                    o = dout("dbg_" + nm, shape, dt)
                    fw.dma(sp, None, o, t_, ap_, sem="st")
                    fw.barrier()
                    return True
                return False
            if cutG("g1"):
                return nc, names, dbg_out
            for c0 in (4, 12):
                v_ = G[:, :, c0:c0 + 4]
                act.emit(lambda: nc.scalar.activation(out=v_, in_=v_, func=AF.Exp, scale=-1.0), r=[G], w=[G])
                act.emit(lambda: nc.scalar.activation(out=v_, in_=v_, func=AF.Ln, bias=1.0, scale=1.0), r=[G], w=[G])
                dve.emit(lambda: nc.vector.tensor_scalar(out=v_, in0=v_, scalar1=-1.0, scalar2=None, op0=ALU.mult), r=[G], w=[G])
            if cutG("g2"):
                return nc, names, dbg_out
            for i in range(NTT):
                p_ = pg[i % 2]
                pv_ = pgv[i % 2]
                for j, l_ in enumerate((triF, triB, ones)):
                    pe.emit(lambda: nc.tensor.matmul(pv_[:, j, :], lhsT=l_[:], rhs=G[:, i, :], start=True, stop=True),
                            r=[l_, G], w=[p_], inc=(j == 2))
                dve.emit(lambda: nc.vector.tensor_copy(out=CS[:, i, :, :], in_=pv_), r=[p_], w=[CS])
            if cutG("g3"):
                return nc, names, dbg_out
            dve.emit(lambda: nc.vector.tensor_tensor(out=CB8[:, :, 0:4], in0=G[:, :, 0:4], in1=CS[:, :, 0, 4:8], op=ALU.subtract), r=[G, CS], w=[CB8])
            dve.emit(lambda: nc.vector.tensor_tensor(out=CB8[:, :, 4:8], in0=G[:, :, 8:12], in1=CS[:, :, 1, 12:16], op=ALU.subtract), r=[G, CS], w=[CB8])
            dve.emit(lambda: nc.vector.tensor_copy(out=TOT8[:, :, 0:4], in_=CS[:, :, 2, 4:8]), r=[CS], w=[TOT8])
            dve.emit(lambda: nc.vector.tensor_copy(out=TOT8[:, :, 4:8], in_=CS[:, :, 2, 12:16]), r=[CS], w=[TOT8])
            if cutG("g4"):
                return nc, names, dbg_out
            act.emit(lambda: nc.scalar.activation(out=EB8[:, :, 0:4], in_=CS[:, :, 0, 4:8], func=AF.Exp), r=[CS], w=[EB8])
            act.emit(lambda: nc.scalar.activation(out=EB8[:, :, 4:8], in_=CS[:, :, 1, 12:16], func=AF.Exp), r=[CS], w=[EB8])
            if cutG("g5"):
                return nc, names, dbg_out
            dve.emit(lambda: nc.vector.tensor_tensor(out=W8[:], in0=CB8[:], in1=TOT8[:], op=ALU.add), r=[CB8, TOT8], w=[W8])
            act.emit(lambda: nc.scalar.activation(out=W8[:], in_=W8[:], func=AF.Exp), r=[W8], w=[W8])
            act.emit(lambda: nc.scalar.activation(out=EL8[:], in_=TOT8[:], func=AF.Exp), r=[TOT8], w=[EL8])
            if cutG("g6"):
                return nc, names, dbg_out
            if stage == 3 and dbg == "gates":
                o = dout("dbg_G", [128, NTT * 16])
                fw.dma(sp, None, o, G, G[:].rearrange("p a b -> p (a b)"), sem="st")
                o = dout("dbg_CS", [128, NTT * 48])
                fw.dma(sp, None, o, CS, CS[:].rearrange("p a b c -> p (a b c)"), sem="st")
                fw.barrier()
                return nc, names, dbg_out

            QT = sb("QT", [128, 2, NTOK], BF16)
            KT = sb("KT", [128, 2, NTOK], BF16)
            Kt = sb("Kt", [128, NTT, 256], BF16)
            V1 = sb("V1", [128, NTT, 258], BF16)
            SO = sb("SO", [128, NT, 256], BF16)
            SCR = sb("SCR", [128, 2, NTOK])
            class _V:
                def __init__(s_, par, fn):
                    s_.par, s_.fn = par, fn
                def __getitem__(s_, idx):
                    return s_.fn(s_.par.t)[idx]
            P32v = _V(SCR, lambda t_: t_[:, 0, :])
            C32v = _V(SCR, lambda t_: t_[:, 1, :])
            HFv = _V(SCR, lambda t_: t_[:].rearrange("p a b -> p (a b)")[:, 0:NT * 256].rearrange("p (a b) -> p a b", a=NT))
            CT32 = sb("CT32", [128, 2, 257])
            CTb = sb("CTb", [128, 2, 258], BF16)
            AT = [sb("AT%d" % i, [128, 128]) for i in range(2)]
            PT = [sb("PT%d" % i, [128, 128], BF16) for i in range(2)]
            TA = [sb("TA%d" % i, [128, 257]) for i in range(2)]
            NN = [sb("NN%d" % i, [128, 257]) for i in range(2)]
            WV = [sb("WV%d" % i, [128, 258], BF16) for i in range(2)]
            HH = [sb("HH%d" % i, [128, 256]) for i in range(2)]
            YM = [sb("YM%d" % i, [128, 256], BF16) for i in range(2)]
            sm = [sb("sm%d" % i, [128, 16]) for i in range(2)]
            bst = [sb("bst%d" % i, [128, 6]) for i in range(2)]
            pSB_t = ph.enter_context(nc.psum_tensor("ps_pSB", [128, 512], F32))
            pSB = T(pSB_t)
            pSTv = pSB_t[:, 0:128]
            pBRv = pSB_t[:, 128:256]
            pNA = psm("pNA", [128, 512])
            pNB = psm("pNB", [128, 512])
            pCL = psm("pCL", [128, 2, 512])
            pKb = psm("pKb", [128, 4, 2, 128], BF16)
            dve.emit(lambda: nc.vector.memset(V1[:], 1.0), w=[V1])
            n_big = [0]
            n_it = [0]

            def ln_rows(src_t, src_ap, k):
                s_ = sm[k]
                b_ = bst[k]
                dve.emit(lambda: nc.vector.bn_stats(out=b_[:], in_=src_ap), r=[src_t], w=[b_])
                dve.emit(lambda: nc.vector.bn_aggr(out=s_[:, 0:2], in_=b_[:]), r=[b_], w=[s_])
                dve.emit(lambda: nc.vector.tensor_scalar(out=s_[:, 2:3], in0=s_[:, 1:2], scalar1=EPS, scalar2=None, op0=ALU.add), r=[s_], w=[s_])
                act.emit(lambda: nc.scalar.activation(out=s_[:, 3:4], in_=s_[:, 2:3], func=AF.Sqrt), r=[s_], w=[s_])
                dve.emit(lambda: nc.vector.reciprocal(out=s_[:, 4:5], in_=s_[:, 3:4]), r=[s_], w=[s_])
                dve.emit(lambda: nc.vector.scalar_tensor_tensor(out=s_[:, 5:6], in0=s_[:, 0:1], scalar=-1.0, in1=s_[:, 4:5],
                                                                op0=ALU.mult, op1=ALU.mult), r=[s_], w=[s_])
                return s_[:, 4:5], s_[:, 5:6]

            tgroups = [(0, 512), (512, 512), (1024, 512), (1536, 512), (2048, 256)]
            for h in range(4 if stage >= 3 else 0):
                sq = load_w(h * 256)
                sk = load_w(1024 + h * 256)
                sv_ = load_w(2048 + h * 256)
                so_ = load_w(3072 + h * 256)
                zf_issue(3 if h < 3 else 99)
                for isk, (s_, dst) in enumerate(((sq, QT), (sk, KT))):
                    for ec in range(2):
                        for (t0, n) in tgroups:
                            p_ = pbig[n_big[0] % 2]
                            n_big[0] += 1
                            for kc in range(16):
                                pe.emit(lambda: nc.tensor.matmul(p_[:, 0:n], lhsT=s_[:, kc, ec * 128:(ec + 1) * 128], rhs=hxT[:, kc, t0:t0 + n],
                                                                 start=(kc == 0), stop=(kc == 15)), r=[s_, hxT], w=[p_], inc=(kc == 15))
                            act.emit(lambda: nc.scalar.copy(out=P32v[:, t0:t0 + n], in_=p_[:, 0:n]), r=[p_], w=[SCR])
                        ci = isk * 8 + h * 2 + ec
                        dve.emit(lambda: nc.vector.tensor_scalar(out=C32v[:], in0=P32v[:], scalar1=cw[:, 1, ci:ci + 1], scalar2=cb[:, ci:ci + 1],
                                                                 op0=ALU.mult, op1=ALU.add), r=[SCR, cw, cb], w=[SCR])
                        for (a0, a1) in ((0, 2048), (2048, 2304)):
                            dve.emit(lambda: nc.vector.scalar_tensor_tensor(out=C32v[:, a0 + 1:a1], in0=P32v[:, a0:a1 - 1], scalar=cw[:, 0, ci:ci + 1],
                                                                            in1=C32v[:, a0 + 1:a1], op0=ALU.mult, op1=ALU.add), r=[SCR, cw], w=[SCR])
                            dve.emit(lambda: nc.vector.scalar_tensor_tensor(out=C32v[:, a0:a1 - 1], in0=P32v[:, a0 + 1:a1], scalar=cw[:, 2, ci:ci + 1],
                                                                            in1=C32v[:, a0:a1 - 1], op0=ALU.mult, op1=ALU.add), r=[SCR, cw], w=[SCR])
                        if isk == 0:
                            act.emit(lambda: nc.scalar.activation(out=QT[:, ec, :], in_=C32v[:], func=AF.Silu), r=[SCR], w=[QT])
                        else:
                            act.emit(lambda: nc.scalar.activation(out=C32v[:], in_=C32v[:], func=AF.Silu), r=[SCR], w=[SCR])
                            act.emit(lambda: nc.scalar.activation(out=KT[:, ec, :], in_=C32v[:], func=AF.Copy, scale=0.0625),
                                      r=[SCR], w=[KT])
                if cut("h1", QT, QT[:].rearrange("p a b -> p (a b)"), [128, 2 * NTOK], BF16):
                    return nc, names, dbg_out
                if cut("h1k", KT, KT[:].rearrange("p a b -> p (a b)"), [128, 2 * NTOK], BF16):
                    return nc, names, dbg_out
                for i in range(NTT):
                    pb = pbig[n_big[0] % 2]
                    n_big[0] += 1
                    for kc in range(16):
                        pe.emit(lambda: nc.tensor.matmul(pb[:, 0:256], lhsT=hxT[:, kc, i * 128:(i + 1) * 128], rhs=sv_[:, kc, :],
                                                         start=(kc == 0), stop=(kc == 15)), r=[hxT, sv_], w=[pb], inc=(kc == 15))
                    if i < NT:
                        po_ = (pNA, pNB)[i % 2]
                        for kc in range(16):
                            pe.emit(lambda: nc.tensor.matmul(po_[:, 0:256], lhsT=hxT[:, kc, i * 128:(i + 1) * 128], rhs=so_[:, kc, :],
                                                             start=(kc == 0), stop=(kc == 15)), r=[hxT, so_], w=[po_], inc=(kc == 15))
                        act.emit(lambda: nc.scalar.activation(out=SO[:, i, :], in_=po_[:, 0:256], func=AF.Sigmoid), r=[po_], w=[SO])
                    if "v" not in os.environ.get("KSKIP", ""):
                        dve.emit(lambda: nc.vector.tensor_copy(out=V1[:, i, 0:256], in_=pb[:, 0:256]), r=[pb], w=[V1])
                if cut("h2a", SO, SO[:].rearrange("p a b -> p (a b)"), [128, NT * 256], BF16):
                    return nc, names, dbg_out
                for i0 in range(0, NTT, 4):
                    nt4 = min(4, NTT - i0)
                    for j4 in range(nt4):
                        i = i0 + j4
                        for ec in range(2):
                            pe.emit(lambda: nc.tensor.transpose(out=pKb[:, j4, ec, :], in_=KT[:, ec, i * 128:(i + 1) * 128], identity=identb[:]),
                                    r=[KT, identb], w=[pKb], inc=(ec == 1 and j4 == nt4 - 1))
                    act.emit(lambda: nc.scalar.copy(out=Kt[:, i0:i0 + nt4, :], in_=pKb[:, 0:nt4, :, :].rearrange("p a b c -> p a (b c)")), r=[pKb], w=[Kt])

                if cut("h2", V1, V1[:].rearrange("p a b -> p (a b)"), [128, NTT * 258], BF16):
                    return nc, names, dbg_out
                if cut("h2k", Kt, Kt[:].rearrange("p a b -> p (a b)"), [128, NTT * 256], BF16):
                    return nc, names, dbg_out
                for d_ in range(2):
                    order = [16, 17] + list(range(16)) if d_ == 0 else [17, 16] + list(range(15, -1, -1))
                    c8 = h if d_ == 0 else 4 + h
                    gl = 4 + h if d_ == 0 else 12 + h
                    tri_, msk_ = (triF, mF) if d_ == 0 else (triB, mB)
                    dve.emit(lambda: nc.vector.memset(CT32[:], 0.0), w=[CT32])
                    dve.emit(lambda: nc.vector.memset(CTb[:], 0.0), w=[CTb])
                    if d_ == 1 and cut("h3", SCR, HFv[:, :, :], [128, NT, 256]):
                        return nc, names, dbg_out
                    for oi, i in enumerate(order):
                        k2 = n_it[0] % 2
                        n_it[0] += 1
                        tsl = slice(i * 128, (i + 1) * 128)
                        if i < NT:
                            for ec in range(2):
                                pe.emit(lambda: nc.tensor.matmul(pSTv, lhsT=KT[:, ec, tsl], rhs=QT[:, ec, tsl], start=(ec == 0), stop=(ec == 1)),
                                        r=[KT, QT], w=[pSB], inc=False)
                            pe.emit(lambda: nc.tensor.matmul(pBRv, lhsT=G[:, i, gl:gl + 1].to_broadcast([128, 128]), rhs=tri_[:], start=True, stop=False),
                                    r=[G, tri_], w=[pSB], inc=False)
                            pe.emit(lambda: nc.tensor.matmul(pBRv, lhsT=ident[:], rhs=msk_[:], start=False, stop=True),
                                    r=[ident, msk_], w=[pSB])
                            at_, pt_, ta_, nn_ = AT[k2], PT[k2], TA[k2], NN[k2]
                            act.emit(lambda: nc.scalar.activation(out=at_[:], in_=pBRv, func=AF.Exp, bias=CB8[:, i, c8:c8 + 1], scale=1.0),
                                     r=[pSB, CB8], w=[at_])
                            dve.emit(lambda: nc.vector.tensor_tensor(out=pt_[:], in0=at_[:], in1=pSTv, op=ALU.mult), r=[at_, pSB], w=[pt_])
                            for ec in range(2):
                                pe.emit(lambda: nc.tensor.matmul(pNA[:, 0:257], lhsT=QT[:, ec, tsl], rhs=CTb[:, ec, 0:257], start=(ec == 0), stop=(ec == 1)),
                                        r=[QT, CTb], w=[pNA], inc=(ec == 1))
                            pe.emit(lambda: nc.tensor.matmul(pNB[:, 0:257], lhsT=pt_[:], rhs=V1[:, i, 0:257], start=True, stop=True), r=[pt_, V1], w=[pNB])
                            act.emit(lambda: nc.scalar.activation(out=ta_[:], in_=pNA[:, 0:257], func=AF.Copy, scale=EB8[:, i, c8:c8 + 1]),
                                     r=[pNA, EB8], w=[ta_])
                            dve.emit(lambda: nc.vector.tensor_tensor(out=nn_[:], in0=ta_[:], in1=pNB[:, 0:257], op=ALU.add), r=[ta_, pNB], w=[nn_])
                            s_ = sm[k2]
                            dve.emit(lambda: nc.vector.tensor_scalar(out=s_[:, 10:11], in0=nn_[:, 256:257], scalar1=-1.0, scalar2=1.0, op0=ALU.mult, op1=ALU.max), r=[nn_], w=[s_])
                            dve.emit(lambda: nc.vector.tensor_tensor(out=s_[:, 8:9], in0=nn_[:, 256:257], in1=s_[:, 10:11], op=ALU.max), r=[nn_, s_], w=[s_])
                            dve.emit(lambda: nc.vector.reciprocal(out=s_[:, 9:10], in_=s_[:, 8:9]), r=[s_], w=[s_])
                            if d_ == 0:
                                dve.emit(lambda: nc.vector.tensor_scalar(out=HFv[:, i, :], in0=nn_[:, 0:256], scalar1=s_[:, 9:10], scalar2=None, op0=ALU.mult),
                                         r=[nn_, s_], w=[SCR])
                            else:
                                hh_, ym_ = HH[k2], YM[k2]
                                dve.emit(lambda: nc.vector.scalar_tensor_tensor(out=hh_[:], in0=nn_[:, 0:256], scalar=s_[:, 9:10], in1=HFv[:, i, :],
                                                                                op0=ALU.mult, op1=ALU.add), r=[nn_, s_, SCR], w=[hh_])
                                rs, nb = ln_rows(hh_, hh_[:], k2)
                                act.emit(lambda: nc.scalar.activation(out=hh_[:], in_=hh_[:], func=AF.Identity, scale=rs, bias=nb), r=[hh_, s_], w=[hh_])
                                dve.emit(lambda: nc.vector.tensor_tensor(out=hh_[:], in0=hh_[:], in1=mnw[:, h * 256:(h + 1) * 256], op=ALU.mult),
                                          r=[hh_, mnw], w=[hh_])
                                dve.emit(lambda: nc.vector.tensor_tensor(out=ym_[:], in0=hh_[:], in1=SO[:, i, :], op=ALU.mult), r=[hh_, SO], w=[ym_])
                                fw.dma(sp, YD, y_d[tsl, h * 256:(h + 1) * 256], ym_, ym_[:], sem="st")
                        if oi < NTT - 1:
                            wv_ = WV[k2]
                            act.emit(lambda: nc.scalar.activation(out=wv_[:, 0:257], in_=V1[:, i, 0:257], func=AF.Copy, scale=W8[:, i, c8:c8 + 1]),
                                      r=[V1, W8], w=[wv_])
                            for ec in range(2):
                                pe.emit(lambda: nc.tensor.matmul(pCL[:, ec, 0:257], lhsT=Kt[:, i, ec * 128:(ec + 1) * 128], rhs=wv_[:, 0:257], start=True, stop=True),
                                        r=[Kt, wv_], w=[pCL], inc=(ec == 1))
                            dve.emit(lambda: nc.vector.scalar_tensor_tensor(out=CT32[:], in0=CT32[:], scalar=EL8[:, i, c8:c8 + 1], in1=pCL[:, :, 0:257],
                                                                            op0=ALU.mult, op1=ALU.add), r=[CT32, EL8, pCL], w=[CT32])
                            act.emit(lambda: nc.scalar.copy(out=CTb[:, :, 0:257], in_=CT32[:]), r=[CT32], w=[CTb])

            wsb = sb("wsb", [128, 4, 128], BF16)
            fw.dma(pool, wsb, wsb[:], None, wsT_d.rearrange("g q p -> q g p"), sem="wld")
            U = [sb("U%d" % i, [128, 256]) for i in range(2)]
            VG = [sb("VG%d" % i, [128, 256]) for i in range(2)]
            VN = [sb("VN%d" % i, [128, 256], BF16) for i in range(2)]
            for g in range(4 if stage >= 3 else 0):
                su = load_w(4112 + g * 256)
                sg = load_w(5136 + g * 256)
                for i in range(NT):
                    k2 = i % 2
                    pb = pbig[n_big[0] % 2]
                    n_big[0] += 1
                    tsl = slice(i * 128, (i + 1) * 128)
                    for jj, s_ in enumerate((su, sg)):
                        for kc in range(16):
                            pe.emit(lambda: nc.tensor.matmul(pb[:, jj * 256:(jj + 1) * 256], lhsT=hxT[:, kc, tsl], rhs=s_[:, kc, :],
                                                             start=(kc == 0), stop=(kc == 15)), r=[hxT, s_], w=[pb], inc=(kc == 15 and jj == 1))
                    u_, vg_, vn_, ym_ = U[k2], VG[k2], VN[k2], YM[k2]
                    act.emit(lambda: nc.scalar.activation(out=u_[:], in_=pb[:, 0:256], func=AF.Gelu_apprx_tanh), r=[pb], w=[u_])
                    act.emit(lambda: nc.scalar.activation(out=vg_[:], in_=pb[:, 256:512], func=AF.Gelu_apprx_tanh), r=[pb], w=[vg_])
                    rs, nb = ln_rows(vg_, vg_[:], k2)
                    act.emit(lambda: nc.scalar.activation(out=vg_[:], in_=vg_[:], func=AF.Identity, scale=rs, bias=nb), r=[vg_, sm[k2]], w=[vg_])
                    dve.emit(lambda: nc.vector.tensor_tensor(out=vn_[:], in0=vg_[:], in1=cnw[:, g * 256:(g + 1) * 256], op=ALU.mult), r=[vg_, cnw], w=[vn_])
                    pe.emit(lambda: nc.tensor.matmul(pNA[:, 0:256], lhsT=wsb[:, g, :], rhs=vn_[:], start=True, stop=True), r=[wsb, vn_], w=[pNA])
                    dve.emit(lambda: nc.vector.scalar_tensor_tensor(out=ym_[:], in0=pNA[:, 0:256], scalar=bsT[:, g:g + 1], in1=u_[:],
                                                                    op0=ALU.add, op1=ALU.mult), r=[pNA, bsT, u_], w=[ym_])
                    fw.dma(sp, YD, y_d[tsl, 1024 + g * 256:1024 + (g + 1) * 256], ym_, ym_[:], sem="st")
            fw.barrier()
        if stage == 3:
            o = dout("dbg_y", [2048, D], BF16)
            with nc.sbuf_tensor("ytmp", [128, 16, D], BF16) as yt_:
                YT_ = T(yt_)
                fw.dma(sp, YT_, yt_[:], YD, y_d.rearrange("(a p) d -> p a d", p=128))
                fw.dma(sp, None, o.rearrange("(a p) d -> p a d", p=128), YT_, yt_[:], sem="st")
                fw.barrier()
            return nc, names, dbg_out

        SLT = sbG("SLT", [128, NT, 2], I32)
        GT = sbG("GT", [128, NT, 2])
        with contextlib.ExitStack() as ph:
            sb, psm = mk(ph, "sb"), mk(ph, "ps")
            wo = sb("wo", [128, 16, D], BF16)
            wov = wout_d.rearrange("(kc p) n -> p kc n", p=128)
            for q4 in range(4):
                fw.dma(pool, wo, wo[:, q4 * 4:(q4 + 1) * 4, :], None, wov[:, q4 * 4:(q4 + 1) * 4, :], sem="wld")
            g1b = sb("g1b", [128, D])
            l1w = sb("l1w", [128, D])
            l1b = sb("l1b", [128, D])
            A2 = sb("A2", [128, D])
            B2 = sb("B2", [128, D])
            fw.dma(sp, g1b, g1b[:], MOD, mod_d[0, 2 * D:3 * D].partition_broadcast(128))
            fw.dma(sp, l1w, l1w[:], None, ln1w_d.partition_broadcast(128))
            fw.dma(sp, l1b, l1b[:], None, ln1b_d.partition_broadcast(128))
            fw.dma(sp, A2, A2[:], MOD, mod_d[0, 4 * D:5 * D].partition_broadcast(128))
            fw.dma(sp, B2, B2[:], MOD, mod_d[0, 3 * D:4 * D].partition_broadcast(128))
            dve.emit(lambda: nc.vector.tensor_scalar(out=A2[:], in0=A2[:], scalar1=1.0, scalar2=None, op0=ALU.add), r=[A2], w=[A2])
            z = sb("z", [128, D])
            t8 = z
            dve.emit(lambda: nc.vector.tensor_tensor(out=t8[:], in0=l1b[:], in1=A2[:], op=ALU.mult), r=[l1b, A2], w=[t8])
            dve.emit(lambda: nc.vector.tensor_tensor(out=B2[:], in0=B2[:], in1=t8[:], op=ALU.add), r=[B2, t8], w=[B2])
            dve.emit(lambda: nc.vector.tensor_tensor(out=A2[:], in0=A2[:], in1=l1w[:], op=ALU.mult), r=[A2, l1w], w=[A2])
            rwT = sb("rwT", [128, 16, 36])
            fw.dma(sp, rwT, rwT[:], None, rw_d.rearrange("(kc p) n -> p kc n", p=128))
            rbb = sb("rbb", [128, 36])
            fw.dma(sp, rbb, rbb[:], None, rb_d.partition_broadcast(128))
            ecap = sb("ecap", [128, 32])
            fw.dma(sp, ecap, ecap[:], None, ecap_d.partition_broadcast(128))
            CNT = sb("CNT", [128, 32])
            dve.emit(lambda: nc.vector.memset(CNT[:], 0.0), w=[CNT])

            yt = [sb("yt%d" % i, [128, D], BF16) for i in range(2)]
            yT = [sb("yT%d" % i, [128, 16, 128], BF16) for i in range(2)]
            xt = [sb("xt%d" % i, [128, D]) for i in range(2)]
            ptl = [sb("ptl0", [128, D])] * 2
            x1 = [sb("x1_0", [128, D])] * 2
            h2f = sb("h2f", [128, D])
            h2b = [sb("h2b%d" % i, [128, D], BF16) for i in range(2)]
            h2T = sb("h2T", [128, 16, 128])
            bst4 = sb("bst4", [128, 4, 6])
            sm4 = [sb("sm4_%d" % i, [128, 16]) for i in range(2)]
            R = [sb("R%d" % i, [128, 256]) for i in range(2)]
            po = psm("po", [128, 4, 512])
            ptb = [psm("ptb%d" % k_, [128, 4, 128], BF16) for k_ in range(2)]
            ptf = psm("ptf", [128, 4, 128])
            pr = psm("pr", [128, 3, 64])

            def loads(i):
                a, b_, c_ = xt[i % 2], ptl[i % 2], yt[i % 2]
                fw.dma(sp, c_, c_[:], YD, y_d[i * 128:(i + 1) * 128, :])
                fw.dma(sp, a, a[:], None, x_d[i * 128:(i + 1) * 128, :])
                fw.dma(sp, b_, b_[:], None, pos_d[i * 128:(i + 1) * 128, :])

            def p4_front(i):
                k2 = i % 2
                a, b_, c_, yT_ = xt[k2], ptl[k2], yt[k2], yT[k2]
                dve.emit(lambda: nc.vector.tensor_tensor(out=a[:], in0=a[:], in1=b_[:], op=ALU.add), r=[a, b_], w=[a])
                if i + 1 < NT:
                    loads(i + 1)
                for g4 in range(4):
                    p_ = ptb[g4 % 2]
                    for q4 in range(4):
                        kc = g4 * 4 + q4
                        pe.emit(lambda: nc.tensor.transpose(out=p_[:, q4, :], in_=c_[:, kc * 128:(kc + 1) * 128], identity=identb[:]),
                                r=[c_, identb], w=[p_], inc=(q4 == 3))
                    (act if g4 % 2 else dve).emit(
                        (lambda: nc.scalar.copy(out=yT_[:, g4 * 4:(g4 + 1) * 4, :], in_=p_[:])) if g4 % 2 else
                        (lambda: nc.vector.tensor_copy(out=yT_[:, g4 * 4:(g4 + 1) * 4, :], in_=p_[:])), r=[p_], w=[yT_])
                for cg in range(4):
                    for kc in range(16):
                        pe.emit(lambda: nc.tensor.matmul(po[:, cg, :], lhsT=yT_[:, kc, :], rhs=wo[:, kc, cg * 512:(cg + 1) * 512],
                                                         start=(kc == 0), stop=(kc == 15)), r=[yT_, wo], w=[po], inc=(kc == 15 and cg == 3))

            def p4_mid(i):
                k2 = i % 2
                a, b_, c_, yT_ = xt[k2], ptl[k2], yt[k2], yT[k2]
                s_ = sm4[k2]
                x1_ = x1[k2]
                hb = h2b[k2]
                for cg in range(4):
                    cs = slice(cg * 512, (cg + 1) * 512)
                    dve.emit(lambda: nc.vector.tensor_tensor(out=z[:, cs], in0=po[:, cg, :], in1=g1b[:, cs], op=ALU.mult), r=[po, g1b], w=[z])
                dve.emit(lambda: nc.vector.scalar_tensor_tensor(out=z[:], in0=a[:], scalar=ALPHA, in1=z[:], op0=ALU.mult, op1=ALU.add), r=[a, z], w=[z])
                s_ = sm4[k2]
                for cg in range(4):
                    dve.emit(lambda: nc.vector.bn_stats(out=bst4[:, cg, :], in_=z[:, cg * 512:(cg + 1) * 512]), r=[z], w=[bst4])
                dve.emit(lambda: nc.vector.bn_aggr(out=s_[:, 0:2], in_=bst4[:].rearrange("p a b -> p (a b)")), r=[bst4], w=[s_])
                dve.emit(lambda: nc.vector.tensor_scalar(out=s_[:, 2:3], in0=s_[:, 1:2], scalar1=EPS, scalar2=None, op0=ALU.add), r=[s_], w=[s_])
                act.emit(lambda: nc.scalar.activation(out=s_[:, 3:4], in_=s_[:, 2:3], func=AF.Sqrt), r=[s_], w=[s_])
                dve.emit(lambda: nc.vector.reciprocal(out=s_[:, 4:5], in_=s_[:, 3:4]), r=[s_], w=[s_])
                dve.emit(lambda: nc.vector.scalar_tensor_tensor(out=s_[:, 5:6], in0=s_[:, 0:1], scalar=-1.0, in1=s_[:, 4:5], op0=ALU.mult, op1=ALU.mult), r=[s_], w=[s_])
                act.emit(lambda: nc.scalar.activation(out=z[:], in_=z[:], func=AF.Identity, scale=s_[:, 4:5], bias=s_[:, 5:6]), r=[z, s_], w=[z])
                x1_ = x1[k2]
                dve.emit(lambda: nc.vector.tensor_tensor(out=x1_[:], in0=z[:], in1=l1w[:], op=ALU.mult), r=[z, l1w], w=[x1_])
                dve.emit(lambda: nc.vector.tensor_tensor(out=x1_[:], in0=x1_[:], in1=l1b[:], op=ALU.add), r=[x1_, l1b], w=[x1_])
                fw.dma(sp, X1D, x1_d[i * 128:(i + 1) * 128, :], x1_, x1_[:], sem="st")
                dve.emit(lambda: nc.vector.tensor_tensor(out=h2f[:], in0=z[:], in1=A2[:], op=ALU.mult), r=[z, A2], w=[h2f])
                dve.emit(lambda: nc.vector.tensor_tensor(out=h2f[:], in0=h2f[:], in1=B2[:], op=ALU.add), r=[h2f, B2], w=[h2f])
                hb = h2b[k2]
                act.emit(lambda: nc.scalar.copy(out=hb[:], in_=h2f[:]), r=[h2f], w=[hb])

            def p4_back(i):
                k2 = i % 2
                a, b_, c_, yT_ = xt[k2], ptl[k2], yt[k2], yT[k2]
                hb = h2b[k2]
                for g4 in range(4):
                    for q4 in range(4):
                        kc = g4 * 4 + q4
                        pe.emit(lambda: nc.tensor.transpose(out=ptf[:, q4, :], in_=h2f[:, kc * 128:(kc + 1) * 128], identity=ident[:]),
                                r=[h2f, ident], w=[ptf], inc=(q4 == 3))
                    act.emit(lambda: nc.scalar.copy(out=h2T[:, g4 * 4:(g4 + 1) * 4, :], in_=ptf[:]), r=[ptf], w=[h2T])
                for kc in range(16):
                    pe.emit(lambda: nc.tensor.matmul(pr[:, 0, 0:36], lhsT=h2T[:, kc, :], rhs=rwT[:, kc, :], start=(kc == 0), stop=(kc == 15)),
                            r=[h2T, rwT], w=[pr], inc=(kc == 15))
                r_ = R[k2]
                L = r_[:, 0:36]
                vts = lambda o_, i0, s1, op0, s2=None, op1=None: dve.emit(
                    lambda: nc.vector.tensor_scalar(out=o_, in0=i0, scalar1=s1, scalar2=s2, op0=op0, **({"op1": op1} if op1 is not None else {})), r=[r_], w=[r_])
                vtt = lambda o_, i0, i1, op: dve.emit(lambda: nc.vector.tensor_tensor(out=o_, in0=i0, in1=i1, op=op), r=[r_], w=[r_])
                vstt = lambda o_, i0, s, i1, op0, op1: dve.emit(
                    lambda: nc.vector.scalar_tensor_tensor(out=o_, in0=i0, scalar=s, in1=i1, op0=op0, op1=op1), r=[r_], w=[r_])
                vmax = lambda o_, i_: dve.emit(lambda: nc.vector.reduce_max(out=o_, in_=i_, axis=AX.X), r=[r_], w=[r_])
                vsum = lambda o_, i_: dve.emit(lambda: nc.vector.reduce_sum(out=o_, in_=i_, axis=AX.X), r=[r_], w=[r_])
                dve.emit(lambda: nc.vector.tensor_tensor(out=L, in0=pr[:, 0, 0:36], in1=rbb[:], op=ALU.add), r=[pr, rbb], w=[r_])
                c = lambda k, n=1: r_[:, k:k + n]
                m1, nm1, s1c, pgc = c(40), c(41), c(42), c(43)
                G1 = c(44, 4)
                E4 = c(48, 4)
                L2S = c(52, 8)
                ma, mb_, dd, ed, rr, g0c, g1c = c(60), c(61), c(62), c(63), c(64), c(65), c(66)
                OH1, L2M, OH2 = c(68, 8), c(76, 8), c(84, 8)
                E0, E1, Mm = c(96, 32), c(128, 32), c(160, 32)
                RK, SV, V01 = c(192, 32), c(224, 32), c(36, 4)
                vmax(m1, L[:, 0:4])
                vts(G1, L[:, 0:4], m1, ALU.is_equal)
                vts(nm1, m1, -1.0, ALU.mult)
                act.emit(lambda: nc.scalar.activation(out=E4, in_=L[:, 0:4], func=AF.Exp, bias=nm1, scale=1.0), r=[r_], w=[r_])
                vsum(s1c, E4)
                dve.emit(lambda: nc.vector.reciprocal(out=pgc, in_=s1c), r=[r_], w=[r_])
                vts(L2S, L[:, 4:12], G1[:, 0:1], ALU.mult)
                for g in range(1, 4):
                    vstt(L2S, L[:, 4 + 8 * g:12 + 8 * g], G1[:, g:g + 1], L2S, ALU.mult, ALU.add)
                vmax(ma, L2S)
                vts(OH1, L2S, ma, ALU.is_equal)
                vstt(L2M, OH1, -1e30, L2S, ALU.mult, ALU.add)
                vmax(mb_, L2M)
                vts(OH2, L2M, mb_, ALU.is_equal)
                vtt(dd, mb_, ma, ALU.subtract)
                act.emit(lambda: nc.scalar.activation(out=ed, in_=dd, func=AF.Exp), r=[r_], w=[r_])
                vts(rr, ed, 1.0, ALU.add)
                dve.emit(lambda: nc.vector.reciprocal(out=rr, in_=rr), r=[r_], w=[r_])
                vtt(g0c, pgc, rr, ALU.mult)
                vtt(g1c, g0c, ed, ALU.mult)
                dve.emit(lambda: nc.vector.tensor_copy(out=GT[:, i, 0:1], in_=g0c), r=[r_], w=[GT])
                dve.emit(lambda: nc.vector.tensor_copy(out=GT[:, i, 1:2], in_=g1c), r=[r_], w=[GT])
                for g in range(4):
                    vts(E0[:, g * 8:(g + 1) * 8], OH1, G1[:, g:g + 1], ALU.mult)
                    vts(E1[:, g * 8:(g + 1) * 8], OH2, G1[:, g:g + 1], ALU.mult)
                vtt(Mm, E0, E1, ALU.add)
                pe.emit(lambda: nc.tensor.matmul(pr[:, 1, 0:32], lhsT=triS[:], rhs=Mm, start=True, stop=True), r=[triS, r_], w=[pr], inc=False)
                pe.emit(lambda: nc.tensor.matmul(pr[:, 2, 0:32], lhsT=ones[:], rhs=Mm, start=True, stop=True), r=[ones, r_], w=[pr])
                dve.emit(lambda: nc.vector.tensor_tensor(out=RK, in0=pr[:, 1, 0:32], in1=CNT[:], op=ALU.add), r=[pr, CNT], w=[r_])
                dve.emit(lambda: nc.vector.tensor_tensor(out=CNT[:], in0=pr[:, 2, 0:32], in1=CNT[:], op=ALU.add), r=[pr, CNT], w=[CNT])
                vts(SV, RK, float(CAP), ALU.is_lt)
                dve.emit(lambda: nc.vector.tensor_tensor(out=RK, in0=RK, in1=ecap[:], op=ALU.add), r=[r_, ecap], w=[r_])
                vts(RK, RK, -float(NS), ALU.add)
                vtt(RK, RK, SV, ALU.mult)
                vts(RK, RK, float(NS), ALU.add)
                sf = c(36, 2)
                vtt(E0, E0, RK, ALU.mult)
                vtt(E1, E1, RK, ALU.mult)
                vsum(sf[:, 0:1], E0)
                vsum(sf[:, 1:2], E1)
                dve.emit(lambda: nc.vector.tensor_copy(out=SLT[:, i, :], in_=sf), r=[r_], w=[SLT])
                for k in range(2):
                    fw.dma(pool, XSD, xs_d, hb, hb[:], sem="ind", idx_t=SLT,
                           out_offset=bass.IndirectOffsetOnAxis(ap=SLT[:, i, k:k + 1], axis=0), in_offset=None)

            loads(0)
            p4_front(0)
            for i in range(NT):
                p4_mid(i)
                if i + 1 < NT:
                    p4_front(i + 1)
                p4_back(i)
            fw.barrier()
        if stage == 4:
            o = dout("dbg_x1", [2048, D])
            with nc.sbuf_tensor("xtmp", [128, 4, D], F32) as yt_:
                YT_ = T(yt_)
                for q in range(4):
                    fw.dma(sp, YT_, yt_[:], X1D, x1_d[q * 512:(q + 1) * 512].rearrange("(a p) d -> p a d", p=128))
                    fw.dma(sp, None, o[q * 512:(q + 1) * 512].rearrange("(a p) d -> p a d", p=128), YT_, yt_[:], sem="st")
            o = dout("dbg_slt", [128, NT * 2], I32)
            fw.dma(sp, None, o, SLT, SLT[:].rearrange("p a b -> p (a b)"), sem="st")
            o = dout("dbg_gt", [128, NT * 2])
            fw.dma(sp, None, o, GT, GT[:].rearrange("p a b -> p (a b)"), sem="st")
        if stage == 4:
            fw.barrier()
            return nc, names, dbg_out

        with contextlib.ExitStack() as ph:
            sb, psm = mk(ph, "sb"), mk(ph, "ps")
            NWP = 8
            wp = [sb("wp%d" % i, [128, 8192], BF16) for i in range(NWP)]
            wp_n = [0]
            xs = [sb("xs%d" % i, [128, 4, D], BF16) for i in range(2)]
            xT = sb("xT", [128, 16, CAP], BF16)
            hT = sb("hT", [128, 8, CAP], BF16)
            sg = [sb("sg%d" % i, [128, CAP]) for i in range(2)]
            ye = [sb("ye%d" % i, [128, 1024], BF16) for i in range(2)]

            def load_xs(e):
                x_ = xs[e % 2]
                fw.dma(sp, x_, x_[:], XSD, xs_d[e * CAP:(e + 1) * CAP, :].rearrange("(b p) d -> p b d", p=128))
            ptb = [psm("ptb%d" % k_, [128, 4, 128], BF16) for k_ in range(2)]
            pgu = [psm("pgu%d" % i, [128, CAP]) for i in range(2)]
            py = [psm("py%d" % i, [128, 2, 512]) for i in range(2)]
            n_y = 0

            def wslot(src_ap, kcn, ncol):
                s_ = wp[wp_n[0] % NWP]
                wp_n[0] += 1
                v_ = s_[:, 0:kcn * ncol].rearrange("p (k n) -> p k n", k=kcn)
                sv = src_ap.rearrange("(kc p) n -> p kc n", p=128)
                fw.dma(pool, s_, v_, None, sv)
                return s_, v_

            def load_expert(e):
                ga = wslot(wg_d[e][0:1024, :], 8, 1024)
                ua = wslot(wu_d[e][0:1024, :], 8, 1024)
                gb = wslot(wg_d[e][1024:2048, :], 8, 1024)
                ub = wslot(wu_d[e][1024:2048, :], 8, 1024)
                dl = wslot(wd_d[e][:, 0:1024], 8, 1024)
                dh = wslot(wd_d[e][:, 1024:2048], 8, 1024)
                return ga, ua, gb, ub, dl, dh

            for e in range(NE):
                ga, ua, gb, ub, dl, dh = load_expert(e)
                xs_ = xs[e % 2]
                if e == 0:
                    load_xs(0)
                if e + 1 < NE:
                    load_xs(e + 1)
                nt_ = 0
                for b in range(4):
                    for g4 in range(4):
                        p_ = ptb[nt_ % 2]
                        nt_ += 1
                        for q4 in range(4):
                            kc = g4 * 4 + q4
                            pe.emit(lambda: nc.tensor.transpose(out=p_[:, q4, :], in_=xs_[:, b, kc * 128:(kc + 1) * 128], identity=identb[:]),
                                    r=[xs_, identb], w=[p_], inc=(q4 == 3))
                        (act if nt_ % 2 else dve).emit(
                            (lambda: nc.scalar.copy(out=xT[:, g4 * 4:(g4 + 1) * 4, b * 128:(b + 1) * 128], in_=p_[:])) if nt_ % 2 else
                            (lambda: nc.vector.tensor_copy(out=xT[:, g4 * 4:(g4 + 1) * 4, b * 128:(b + 1) * 128], in_=p_[:])), r=[p_], w=[xT])
                for j in range(8):
                    pg_, pu_ = pgu[0], pgu[1]
                    for kc in range(16):
                        Wg, Wgv = ga if kc < 8 else gb
                        pe.emit(lambda: nc.tensor.matmul(pg_[:], lhsT=Wgv[:, kc % 8, j * 128:(j + 1) * 128], rhs=xT[:, kc, :], start=(kc == 0), stop=(kc == 15)),
                                r=[Wg, xT], w=[pg_], inc=(kc == 15))
                    for kc in range(16):
                        Wu, Wuv = ua if kc < 8 else ub
                        pe.emit(lambda: nc.tensor.matmul(pu_[:], lhsT=Wuv[:, kc % 8, j * 128:(j + 1) * 128], rhs=xT[:, kc, :], start=(kc == 0), stop=(kc == 15)),
                                r=[Wu, xT], w=[pu_], inc=(kc == 15))
                    s_ = sg[j % 2]
                    act.emit(lambda: nc.scalar.activation(out=s_[:], in_=pg_[:], func=AF.Silu), r=[pg_], w=[s_])
                    dve.emit(lambda: nc.vector.tensor_tensor(out=hT[:, j, :], in0=s_[:], in1=pu_[:], op=ALU.mult), r=[s_, pu_], w=[hT])
                for hf, (Wd, Wdv) in enumerate((dl, dh)):
                    for b in range(4):
                        py_ = py[n_y % 2]
                        y_ = ye[n_y % 2]
                        n_y += 1
                        for cg in range(2):
                            for kc in range(8):
                                pe.emit(lambda: nc.tensor.matmul(py_[:, cg, :], lhsT=hT[:, kc, b * 128:(b + 1) * 128], rhs=Wdv[:, kc, cg * 512:(cg + 1) * 512],
                                                                 start=(kc == 0), stop=(kc == 7)), r=[hT, Wd], w=[py_], inc=(kc == 7 and cg == 1))
                        if n_y % 2:
                            act.emit(lambda: nc.scalar.copy(out=y_[:], in_=py_[:].rearrange("p a b -> p (a b)")), r=[py_], w=[y_])
                        else:
                            dve.emit(lambda: nc.vector.tensor_copy(out=y_[:], in_=py_[:].rearrange("p a b -> p (a b)")), r=[py_], w=[y_])
                        fw.dma(sp, YSD, ys_d[e * CAP + b * 128:e * CAP + (b + 1) * 128, hf * 1024:(hf + 1) * 1024], y_, y_[:])
            fw.barrier()

        with contextlib.ExitStack() as ph:
            sb, psm = mk(ph, "sb"), mk(ph, "ps")
            g2b = sb("g2b", [128, D])
            l2w = sb("l2w", [128, D])
            l2b = sb("l2b", [128, D])
            fw.dma(sp, g2b, g2b[:], MOD, mod_d[0, 5 * D:6 * D].partition_broadcast(128))
            fw.dma(sp, l2w, l2w[:], None, ln2w_d.partition_broadcast(128))
            fw.dma(sp, l2b, l2b[:], None, ln2b_d.partition_broadcast(128))
            y0 = [sb("y0_%d" % i, [128, D], BF16) for i in range(2)]
            y1 = [sb("y1_%d" % i, [128, D], BF16) for i in range(2)]
            x1 = [sb("x1_%d" % i, [128, D]) for i in range(2)]
            mo = sb("mo", [128, D])
            ot = [sb("ot%d" % i, [128, D]) for i in range(2)]
            bst4 = sb("bst4", [128, 4, 6])
            sm4 = [sb("sm4_%d" % i, [128, 16]) for i in range(2)]

            def loads6(i):
                k2 = i % 2
                fw.dma(pool, y0[k2], y0[k2][:], YSD, ys_d, sem="ind", idx_t=SLT, out_offset=None,
                       in_offset=bass.IndirectOffsetOnAxis(ap=SLT[:, i, 0:1], axis=0))
                fw.dma(pool, y1[k2], y1[k2][:], YSD, ys_d, sem="ind", idx_t=SLT, out_offset=None,
                       in_offset=bass.IndirectOffsetOnAxis(ap=SLT[:, i, 1:2], axis=0))
                fw.dma(sp, x1[k2], x1[k2][:], X1D, x1_d[i * 128:(i + 1) * 128, :])

            loads6(0)
            for i in range(NT):
                if i + 1 < NT:
                    loads6(i + 1)
                k2 = i % 2
                a0, a1, xx, o_ = y0[k2], y1[k2], x1[k2], ot[k2]
                dve.emit(lambda: nc.vector.tensor_scalar(out=mo[:], in0=a0[:], scalar1=GT[:, i, 0:1], scalar2=None, op0=ALU.mult), r=[a0, GT], w=[mo])
                dve.emit(lambda: nc.vector.scalar_tensor_tensor(out=mo[:], in0=a1[:], scalar=GT[:, i, 1:2], in1=mo[:], op0=ALU.mult, op1=ALU.add),
                         r=[a1, GT, mo], w=[mo])
                dve.emit(lambda: nc.vector.tensor_tensor(out=mo[:], in0=mo[:], in1=g2b[:], op=ALU.mult), r=[mo, g2b], w=[mo])
                dve.emit(lambda: nc.vector.scalar_tensor_tensor(out=mo[:], in0=xx[:], scalar=ALPHA, in1=mo[:], op0=ALU.mult, op1=ALU.add), r=[xx, mo], w=[mo])
                s_ = sm4[k2]
                for cg in range(4):
                    dve.emit(lambda: nc.vector.bn_stats(out=bst4[:, cg, :], in_=mo[:, cg * 512:(cg + 1) * 512]), r=[mo], w=[bst4])
                dve.emit(lambda: nc.vector.bn_aggr(out=s_[:, 0:2], in_=bst4[:].rearrange("p a b -> p (a b)")), r=[bst4], w=[s_])
                dve.emit(lambda: nc.vector.tensor_scalar(out=s_[:, 2:3], in0=s_[:, 1:2], scalar1=EPS, scalar2=None, op0=ALU.add), r=[s_], w=[s_])
                act.emit(lambda: nc.scalar.activation(out=s_[:, 3:4], in_=s_[:, 2:3], func=AF.Sqrt), r=[s_], w=[s_])
                dve.emit(lambda: nc.vector.reciprocal(out=s_[:, 4:5], in_=s_[:, 3:4]), r=[s_], w=[s_])
                dve.emit(lambda: nc.vector.scalar_tensor_tensor(out=s_[:, 5:6], in0=s_[:, 0:1], scalar=-1.0, in1=s_[:, 4:5], op0=ALU.mult, op1=ALU.mult), r=[s_], w=[s_])
                act.emit(lambda: nc.scalar.activation(out=mo[:], in_=mo[:], func=AF.Identity, scale=s_[:, 4:5], bias=s_[:, 5:6]), r=[mo, s_], w=[mo])
                dve.emit(lambda: nc.vector.tensor_tensor(out=o_[:], in0=mo[:], in1=l2w[:], op=ALU.mult), r=[mo, l2w], w=[o_])
                dve.emit(lambda: nc.vector.tensor_tensor(out=o_[:], in0=o_[:], in1=l2b[:], op=ALU.add), r=[o_, l2b], w=[o_])
                fw.dma(sp, None, out_d[i * 128:(i + 1) * 128, :], o_, o_[:], sem="out")
            fw.barrier()
    return nc, names, dbg_out


def _pos_table():
    rows, gw, q = 32, 64, 512
    r = np.repeat(np.arange(rows, dtype=np.float32), gw)
    col = np.tile(np.arange(gw, dtype=np.float32), rows)
    omega = (1.0 / (np.float32(10000.0) ** (np.arange(q, dtype=np.float32) / np.float32(q)))).astype(np.float32)
    ar = r[:, None] * omega
    ac = col[:, None] * omega
    return np.concatenate([np.sin(ar), np.cos(ar), np.sin(ac), np.cos(ac)], -1).astype(np.float32)


def _consts():
    t = np.arange(128)
    c = {}
    c["ident"] = np.eye(128, dtype=np.float32)
    c["triF"] = (t[:, None] <= t[None, :]).astype(np.float32)
    c["triB"] = (t[:, None] >= t[None, :]).astype(np.float32)
    c["triS"] = (t[:, None] < t[None, :]).astype(np.float32)
    c["maskF"] = np.where(t[:, None] <= t[None, :], 0.0, NEG).astype(np.float32)
    c["maskB"] = np.where(t[:, None] >= t[None, :], 0.0, NEG).astype(np.float32)
    c["ecap"] = (np.arange(32) * CAP).astype(np.float32)
    c["pos"] = _pos_table()
    return c


def make_in_map(inp, b, names, consts):
    f = lambda a: np.ascontiguousarray(a, dtype=np.float32)
    m = {}
    m["x"] = f(inp["x"][b])
    m["ctx"] = f(inp["ctx"][b])
    cc = np.stack([inp["c"][b], inp["c_ctx"]], -1)
    m["ccT"] = f(cc.reshape(16, 128, 2).transpose(1, 0, 2))
    m["w_mod"] = f(inp["w_mod"][0])
    m["b_mod"] = f(inp["b_mod"][0])
    m["w_in"] = f(inp["w_in"][0])
    m["conv_wT"] = f(inp["conv_w"][0].reshape(3, 16, 128).transpose(2, 0, 1))
    m["conv_bT"] = f(inp["conv_b"][0].reshape(16, 128).T)
    m["gate_bias"] = f(inp["gate_bias"][0].reshape(16))
    m["mlstm_norm_w"] = f(inp["mlstm_norm_w"][0])
    m["cmlp_norm_w"] = f(inp["cmlp_norm_w"][0])
    m["w_sT"] = f(inp["w_s"][0].transpose(0, 2, 1))
    m["b_sT"] = f(inp["b_s"][0].T)
    if "w_out" in names:
        m["w_out"] = f(inp["w_out"][0])
        m["ln1_w"] = f(inp["ln1_w"][0])
        m["ln1_b"] = f(inp["ln1_b"][0])
        m["rw"] = f(np.concatenate([inp["router1_w"][0], inp["router2_w"][0]], -1))
        m["rb"] = f(np.concatenate([inp["router1_b"][0], inp["router2_b"][0]], -1))
    if "w_gate" in names:
        m["w_gate"] = f(inp["w_gate"][0])
        m["w_up"] = f(inp["w_up"][0])
        m["w_down"] = f(inp["w_down"][0])
        m["ln2_w"] = f(inp["ln2_w"][0])
        m["ln2_b"] = f(inp["ln2_b"][0])
    for k, v in consts.items():
        if k in names:
            m[k] = v
    return {k: v for k, v in m.items() if k in names}


def kernel(**inputs):
    inp = {k: np.asarray(v) for k, v in inputs.items()}
    nc, names, _ = build()
    consts = _consts()
    in_maps = [make_in_map(inp, b, names, consts) for b in range(8)]
    res = run_bass_kernel_spmd(nc, in_maps, core_ids=list(range(8)))
    return np.stack([np.asarray(r["out"], dtype=np.float32) for r in res.results], 0)
```
